# Optimizing a Trainium2 kernel written in Bass

```python
import jax, jax.numpy as jnp
from jax import lax
import numpy as np

D_MODEL = 2048
BATCH = 4
SEQ = 4096
DEPTH = 2

MIX_WIDTH = D_MODEL
RET_HEADS = 4
RET_QK_DIM = D_MODEL // 8
RET_V_DIM = D_MODEL // 8
RET_WIDTH = RET_HEADS * RET_V_DIM
RET_CHUNK = 256
MOBA_HEADS = 8
MOBA_HEAD_DIM = D_MODEL // 16
MOBA_WIDTH = MOBA_HEADS * MOBA_HEAD_DIM
MOBA_BLOCK = 256
MOBA_TOPK = 3
MOBA_QUERY_CHUNK = 16
D_FF = 11 * D_MODEL // 4
CONV_WIDTH = 3
NORM_EPS = 1e-6
NEG_INF = -1e30
IN_SIZES = (RET_HEADS * RET_QK_DIM, RET_HEADS * RET_QK_DIM, RET_WIDTH, RET_WIDTH,
            MOBA_WIDTH, MOBA_WIDTH, MOBA_WIDTH)
IN_COLS = sum(IN_SIZES)
IN_SPLITS = tuple(int(v) for v in np.cumsum(IN_SIZES)[:-1])

kernel_name = "hybrid_retention_moba_convffn"


def rms_norm(x, g):
    xf = x.astype(jnp.float32)
    y = xf * lax.rsqrt(jnp.mean(xf * xf, axis=-1, keepdims=True) + NORM_EPS)
    return (y * g.astype(jnp.float32)).astype(x.dtype)


def pad_to_multiple(a, axis, mult):
    pad = (-a.shape[axis]) % mult
    if pad == 0:
        return a
    widths = [(0, 0)] * a.ndim
    widths[axis] = (0, pad)
    return jnp.pad(a, widths)


def retention_log_decay():
    gamma = 1.0 - 2.0 ** (-5.0 - jnp.arange(RET_HEADS, dtype=jnp.float32))
    return jnp.log(gamma)


def chunkwise_retention(q, k, v):
    B, H, S, dk = q.shape
    dv = v.shape[-1]
    C = RET_CHUNK
    N = S // C
    dt = q.dtype
    log_g = retention_log_decay()
    i = jnp.arange(C, dtype=jnp.float32)
    diff = i[:, None] - i[None, :]
    inner_decay = jnp.where(diff >= 0, jnp.exp(log_g[:, None, None] * jnp.maximum(diff, 0.0)), 0.0)
    q_decay = jnp.exp(log_g[:, None] * (i + 1.0))
    k_decay = jnp.exp(log_g[:, None] * (C - 1.0 - i))
    chunk_decay = jnp.exp(log_g * C)
    qc = q.reshape(B, H, N, C, dk)
    kc = k.reshape(B, H, N, C, dk) * (dk ** -0.5)
    vc = v.reshape(B, H, N, C, dv)
    scores = jnp.einsum('bhncd,bhnsd->bhncs', qc, kc) * inner_decay[:, None].astype(dt)
    inner = jnp.einsum('bhncs,bhnse->bhnce', scores, vc)
    chunk_kv = jnp.einsum('bhnsd,bhnse->nbhde', kc * k_decay[:, None, :, None].astype(dt), vc)
    cdec = chunk_decay[:, None, None].astype(dt)

    def step(state, kv):
        return state * cdec + kv, state

    _, states = lax.scan(step, jnp.zeros((B, H, dk, dv), dt), chunk_kv)
    cross = jnp.einsum('bhncd,nbhde->bhnce', qc, states) * q_decay[:, None, :, None].astype(dt)
    return (inner + cross).reshape(B, H, S, dv)


def alibi_slopes(n):
    return 2.0 ** (-8.0 * (jnp.arange(n, dtype=jnp.float32) + 1.0) / n)


def moba_attention(q, k, v):
    B, H, S, dh = q.shape
    L = MOBA_BLOCK
    NB = S // L
    Qc = MOBA_QUERY_CHUNK
    NC = S // Qc
    top_k = min(MOBA_TOPK, NB)
    scale = dh ** -0.5
    kb = k.reshape(B, H, NB, L, dh)
    vb = v.reshape(B, H, NB, L, dh)
    k_mean = jnp.mean(kb.astype(jnp.float32), axis=3)
    gate = jnp.einsum('bhsd,bhnd->bhsn', q.astype(jnp.float32), k_mean)
    own_block = jnp.arange(S) // L
    fully_past = jnp.arange(NB)[None, :] < own_block[:, None]
    gate = jnp.where(fully_past, gate, -jnp.inf)
    _, sel = lax.top_k(gate, top_k)
    slopes = alibi_slopes(H)
    qs = jnp.moveaxis(q.reshape(B, H, NC, Qc, dh), 2, 0)
    sels = jnp.moveaxis(sel.reshape(B, H, NC, Qc, top_k), 2, 0)
    b_ix = jnp.arange(B)[:, None, None, None]
    h_ix = jnp.arange(H)[None, :, None, None]
    key_off = jnp.arange(L)

    def attend(args):
        c, q_c, sel_c = args
        t = c * Qc + jnp.arange(Qc)
        blk = (c * Qc) // L
        k_own = lax.dynamic_index_in_dim(kb, blk, axis=2, keepdims=False)
        v_own = lax.dynamic_index_in_dim(vb, blk, axis=2, keepdims=False)
        k_sel = kb[b_ix, h_ix, sel_c]
        v_sel = vb[b_ix, h_ix, sel_c]
        dist_own = (t[:, None] - (blk * L + key_off)[None, :]).astype(jnp.float32)
        dist_sel = (t[:, None, None] - (sel_c[..., None] * L + key_off)).astype(jnp.float32)
        logit_own = jnp.einsum('bhqd,bhld->bhql', q_c, k_own).astype(jnp.float32) * scale
        logit_sel = jnp.einsum('bhqd,bhqkld->bhqkl', q_c, k_sel).astype(jnp.float32) * scale
        logit_own = jnp.where(dist_own >= 0, logit_own - slopes[:, None, None] * dist_own, NEG_INF)
        valid = (sel_c < blk)[..., None]
        logit_sel = jnp.where(valid, logit_sel - slopes[:, None, None, None] * dist_sel, NEG_INF)
        logits = jnp.concatenate([logit_own, logit_sel.reshape(B, H, Qc, top_k * L)], axis=-1)
        p = jax.nn.softmax(logits, axis=-1).astype(v.dtype)
        out = (jnp.einsum('bhql,bhld->bhqd', p[..., :L], v_own)
               + jnp.einsum('bhqkl,bhqkld->bhqd', p[..., L:].reshape(B, H, Qc, top_k, L), v_sel))
        return out

    out = lax.map(attend, (jnp.arange(NC), qs, sels))
    return jnp.moveaxis(out, 0, 2).reshape(B, H, S, dh)


def to_heads(a, n_heads):
    B, S, _ = a.shape
    return a.reshape(B, S, n_heads, -1).transpose(0, 2, 1, 3)


def hybrid_mixer(h, w_in, ret_norm_g, q_norm_g, k_norm_g, w_out):
    B, S, _ = h.shape
    proj = h @ w_in
    rq, rk, rv, rg, mq, mk, mv = jnp.split(proj, IN_SPLITS, axis=-1)
    y_r = chunkwise_retention(pad_to_multiple(to_heads(rq, RET_HEADS), 2, RET_CHUNK),
                              pad_to_multiple(to_heads(rk, RET_HEADS), 2, RET_CHUNK),
                              pad_to_multiple(to_heads(rv, RET_HEADS), 2, RET_CHUNK))[:, :, :S]
    y_r = rms_norm(y_r.transpose(0, 2, 1, 3), ret_norm_g)
    y_r = y_r.reshape(B, S, RET_WIDTH) * jax.nn.silu(rg)
    qm = rms_norm(to_heads(mq, MOBA_HEADS), q_norm_g)
    km = rms_norm(to_heads(mk, MOBA_HEADS), k_norm_g)
    vm = to_heads(mv, MOBA_HEADS)
    y_m = moba_attention(pad_to_multiple(qm, 2, MOBA_BLOCK),
                         pad_to_multiple(km, 2, MOBA_BLOCK),
                         pad_to_multiple(vm, 2, MOBA_BLOCK))[:, :, :S]
    y_m = y_m.transpose(0, 2, 1, 3).reshape(B, S, MOBA_WIDTH)
    return jnp.concatenate([y_r, y_m], axis=-1) @ w_out


def conv_ffn(h, w_gate, w_up, conv_w, conv_b, w_down):
    S = h.shape[1]
    a = h @ w_gate
    a_pad = jnp.pad(a, ((0, 0), (CONV_WIDTH - 1, 0), (0, 0)))
    conv = conv_b
    for j in range(CONV_WIDTH):
        conv = conv + conv_w[j] * a_pad[:, j:j + S]
    return (jax.nn.silu(conv) * (h @ w_up)) @ w_down


def setup_inputs(seed: int = 0) -> dict:
    key = jax.random.key(seed)
    ks = jax.random.split(key, 13)
    nrm = jax.random.normal
    f32 = jnp.float32
    return {
        "x": nrm(ks[0], (BATCH, SEQ, D_MODEL), f32),
        "ln1_g": 1.0 + 0.02 * nrm(ks[1], (DEPTH, D_MODEL), f32),
        "w_in": nrm(ks[2], (DEPTH, D_MODEL, IN_COLS), f32) * D_MODEL ** -0.5,
        "ret_norm_g": 1.0 + 0.02 * nrm(ks[3], (DEPTH, RET_HEADS, RET_V_DIM), f32),
        "q_norm_g": 1.0 + 0.02 * nrm(ks[4], (DEPTH, MOBA_HEAD_DIM), f32),
        "k_norm_g": 1.0 + 0.02 * nrm(ks[5], (DEPTH, MOBA_HEAD_DIM), f32),
        "w_out": nrm(ks[6], (DEPTH, MIX_WIDTH, D_MODEL), f32) * MIX_WIDTH ** -0.5,
        "ln2_g": 1.0 + 0.02 * nrm(ks[7], (DEPTH, D_MODEL), f32),
        "w_gate": nrm(ks[8], (DEPTH, D_MODEL, D_FF), f32) * D_MODEL ** -0.5,
        "w_up": nrm(ks[9], (DEPTH, D_MODEL, D_FF), f32) * D_MODEL ** -0.5,
        "conv_w": nrm(ks[10], (DEPTH, CONV_WIDTH, D_FF), f32) * CONV_WIDTH ** -0.5,
        "conv_b": 0.02 * nrm(ks[11], (DEPTH, D_FF), f32),
        "w_down": nrm(ks[12], (DEPTH, D_FF, D_MODEL), f32) * D_FF ** -0.5,
    }


def reference(x, ln1_g, w_in, ret_norm_g, q_norm_g, k_norm_g, w_out, ln2_g, w_gate, w_up, conv_w, conv_b, w_down):
    for l in range(DEPTH):
        h = rms_norm(x, ln1_g[l])
        x = x + hybrid_mixer(h, w_in[l], ret_norm_g[l], q_norm_g[l], k_norm_g[l], w_out[l])
        h = rms_norm(x, ln2_g[l])
        x = x + conv_ffn(h, w_gate[l], w_up[l], conv_w[l], conv_b[l], w_down[l])
    return x
```

```python
class Res:
    __slots__ = ("name", "w", "r", "dsem", "dcount", "last_dma")

    def __init__(self, name):
        self.name = name
        self.w = {}
        self.r = {}
        self.dsem = None
        self.dcount = 0
        self.last_dma = None


class Sched:
    ENG = ("pe", "dve", "act", "pool", "sp")

    def __init__(self, nc, sems):
        self.nc = nc
        self.sems = sems
        self.ops = {e: [] for e in self.ENG}
        self.esem = {e: self.sems.pop() for e in self.ENG}
        self.cnt = {e: 0 for e in self.ENG}
        self.waited = {e: {} for e in self.ENG}
        self.nwaits = 0

    def res(self, name, dma=False):
        r = Res(name)
        if dma:
            if not hasattr(self, "dpool"):
                self.dpool = []
                self.live = []
            if self.dpool:
                r.dsem, r.dcount = self.dpool.pop()
            else:
                r.dsem, r.dcount = self.sems.pop(), 0
            self.live.append(r)
        return r

    def release_all(self):
        for r in getattr(self, "live", []):
            self.dpool.append((r.dsem, r.dcount))
        self.live = []

    def barrier(self):
        evs = {}
        for e in self.ENG:
            if self.cnt[e] > 0:
                self._merge(evs, (self.esem[e], self.cnt[e]))
        for r in getattr(self, "live", []):
            if r.last_dma is not None:
                self._merge(evs, r.last_dma)
        for e in self.ENG:
            wl = self._filter(e, dict(evs))
            self.ops[e].append((wl, None, None))

    @staticmethod
    def _merge(d, ev):
        k = id(ev[0])
        if k not in d or d[k][1] < ev[1]:
            d[k] = ev

    def _collect(self, reads, writes):
        waits = {}
        for r in reads:
            for ev in r.w.values():
                self._merge(waits, ev)
        for w in writes:
            for ev in w.w.values():
                self._merge(waits, ev)
            for ev in w.r.values():
                self._merge(waits, ev)
        return waits

    def _filter(self, eng, waits, skip_self=False):
        out = []
        wd = self.waited[eng]
        for k, (sem, val) in waits.items():
            if skip_self and sem is self.esem[eng]:
                continue
            if wd.get(k, 0) >= val:
                continue
            wd[k] = val
            out.append((sem, val))
        self.nwaits += len(out)
        return out

    def op(self, eng, fn, reads=(), writes=()):
        waits = self._collect(reads, writes)
        wl = self._filter(eng, waits, skip_self=(eng == "pe"))
        self.cnt[eng] += 1
        ev = (self.esem[eng], self.cnt[eng])
        for r in reads:
            self._merge(r.r, ev)
        for w in writes:
            w.w = {id(ev[0]): ev}
            w.r = {}
        self.ops[eng].append((wl, fn, (self.esem[eng], 1)))

    def dma(self, eng, out, in_, slot, reads=(), writes=(), **kw):
        assert slot.dsem is not None, slot.name
        waits = self._collect(reads, writes)
        if slot.last_dma is not None:
            self._merge(waits, slot.last_dma)
        wl = self._filter(eng, waits)
        slot.dcount += 16
        ev = (slot.dsem, slot.dcount)
        slot.last_dma = ev
        for r in reads:
            self._merge(r.r, ev)
        for w in writes:
            if w is slot:
                w.w = {id(ev[0]): ev}
            else:
                self._merge(w.w, ev)
            w.r = {}

        def fn(e, out=out, in_=in_, kw=kw):
            return e.dma_start(out=out, in_=in_, **kw)

        self.ops[eng].append((wl, fn, (slot.dsem, 16)))

    def custom(self, eng, fn, sem_res, reads=(), writes=(), inc=16):
        waits = self._collect(reads, writes)
        if sem_res.last_dma is not None:
            self._merge(waits, sem_res.last_dma)
        wl = self._filter(eng, waits)
        sem_res.dcount += inc
        ev = (sem_res.dsem, sem_res.dcount)
        sem_res.last_dma = ev
        for r in reads:
            self._merge(r.r, ev)
        for w in writes:
            w.w = {id(ev[0]): ev}
            w.r = {}
        self.ops[eng].append((wl, fn, (sem_res.dsem, inc)))

    def final_wait(self, eng, resources):
        waits = self._collect(resources, resources)
        wl = self._filter(eng, waits)
        self.ops[eng].append((wl, None, None))

    def emit(self, block):
        names = {"pe": "tensor", "dve": "vector", "act": "scalar", "pool": "gpsimd", "sp": "sync"}
        for e in self.ENG:
            ops = self.ops[e]
            self.ops[e] = []

            def body(engine, ops=ops):
                for wl, fn, inc in ops:
                    for sem, val in wl:
                        engine.wait_ge(sem, val)
                    if fn is not None:
                        ins = fn(engine)
                        ins.then_inc(inc[0], inc[1])

            getattr(block, names[e])(body)
import numpy as np
import ml_dtypes
from contextlib import ExitStack
import concourse.bass as bass
import concourse.mybir as mybir
from concourse.bass_utils import run_bass_kernel_spmd

F32 = mybir.dt.float32
BF16 = mybir.dt.bfloat16
AF = mybir.ActivationFunctionType
ALU = mybir.AluOpType
AX = mybir.AxisListType

NCORES = 4
T = 4096
D = 2048
KC = 16
DFF = 5632
FT = 44
DEPTH = 2
EPS = 1e-6
GAMMA = [1.0 - 2.0 ** (-5.0 - h) for h in range(4)]
SLOPES = [2.0 ** (-(h + 1)) for h in range(8)]
NEG = -30000.0


def host_consts():
    c = {}
    bf = ml_dtypes.bfloat16
    c["ident"] = np.eye(128, dtype=np.float32).astype(bf)
    c["ones"] = np.ones((128, 128), np.float32).astype(bf)
    c["identf"] = np.eye(128, dtype=np.float32)
    p = np.arange(128)
    kd = np.zeros((128, 2, 4), np.float32)
    qd = np.zeros((128, 2, 4), np.float32)
    dec = np.zeros((128, 4, 2, 256), np.float32)
    cc = np.arange(256)
    for h in range(4):
        lg = np.log(np.float32(GAMMA[h])).astype(np.float32)
        for par in range(2):
            pos = par * 128 + p
            kd[:, par, h] = np.exp(lg * (255.0 - pos)) / 16.0
            qd[:, par, h] = np.exp(lg * (pos + 1.0))
            diff = cc[None, :] - pos[:, None]
            dec[:, h, par, :] = np.where(diff >= 0, np.exp(lg * np.maximum(diff, 0)), 0.0) / 16.0
    c["kd"] = kd
    c["qd"] = qd
    c["dec"] = dec
    qt = np.arange(32)
    slot = np.arange(16)
    valid = (slot[None, :] < (qt // 2)[:, None]).astype(np.float32)
    own = (slot[None, :] == (qt // 2)[:, None]).astype(np.float32)
    c["negb"] = np.broadcast_to(np.where(valid > 0, 0.0, -1e30).astype(np.float32)[None], (128, 32, 16)).copy()
    c["validf"] = np.broadcast_to(valid[None], (128, 32, 16)).copy()
    c["own1h"] = np.broadcast_to(own[None], (128, 32, 16)).copy()
    bl = np.zeros((128, 32, 128), np.float32)
    for j in range(32):
        bl[j // 2, j, :] = 1.0
        bl[32, j, :] = 1.0
        bl[33, j, :] = 1.0
        bl[34, j, :] = np.arange(128)
        bl[35, j, :] = j
    c["bl"] = bl.astype(bf)
    brc = np.zeros((8, 4, T), np.float32)
    t = np.arange(T)
    for h in range(8):
        s = SLOPES[h]
        brc[h, 0] = -s * (t % 256)
        brc[h, 1] = -s * 256.0 * (t // 256)
        brc[h, 2] = s
        brc[h, 3] = s * 128.0
    c["brc"] = brc.astype(bf)
    tri = np.zeros((128, 2, 256), np.float32)
    for a in range(2):
        tri[:, a, :] = np.where((p[:, None] + 128 * a) > cc[None, :], NEG, 0.0)
    c["tri"] = tri.astype(bf)
    return c


CONST_SHAPES = {
    "identf": ([128, 128], F32), "ident": ([128, 128], BF16), "ones": ([128, 128], BF16), "kd": ([128, 2, 4], F32), "qd": ([128, 2, 4], F32),
    "dec": ([128, 4, 2, 256], F32), "negb": ([128, 32, 16], F32), "validf": ([128, 32, 16], F32),
    "own1h": ([128, 32, 16], F32), "bl": ([128, 32, 128], BF16), "brc": ([8, 4, T], BF16), "tri": ([128, 2, 256], BF16),
}

IN_SHAPES = {
    "x": [T, D], "ln1_g": [DEPTH, D], "w_in": [DEPTH, D, 7168], "ret_norm_g": [DEPTH, 1024], "q_norm_g": [DEPTH, 128],
    "k_norm_g": [DEPTH, 128], "w_out": [DEPTH, D, D], "ln2_g": [DEPTH, D], "w_gate": [DEPTH, D, DFF],
    "w_up": [DEPTH, D, DFF], "conv_w": [DEPTH, 3, DFF], "conv_b": [DEPTH, DFF], "w_down": [DEPTH, DFF, D],
}

SCRATCH = {
    "RQT": ([8, 128, T], BF16), "RKT": ([8, 128, T], BF16), "RK": ([T, 1024], BF16), "RV": ([T, 1024], BF16),
    "SG": ([T, 1024], F32), "MQT": ([8, 128, T], BF16), "MKT": ([8, 128, T], BF16), "MV": ([T, 1024], BF16),
    "YTD": ([16, 128, T], BF16), "XR": ([T, D], F32), "XO": ([T, D], F32), "GT": ([FT, 128, T], BF16),
}


class Ctx:
    pass


DBG = {}


def build(n_layers=DEPTH, stop_after=None, debug=False):
    nc = bass.Bass("TRN2", target_bir_lowering=False)
    G = Ctx()
    G.nc = nc
    G.inp = {k: nc.dram_tensor(k, v, F32, kind="ExternalInput").ap() for k, v in IN_SHAPES.items()}
    G.cst = {k: nc.dram_tensor("c_" + k, v[0], v[1], kind="ExternalInput").ap() for k, v in CONST_SHAPES.items()}
    G.out = nc.dram_tensor("out", [T, D], F32, kind="ExternalOutput").ap()
    G.scr = {k: nc.dram_tensor("s_" + k, v[0], v[1], kind=("ExternalOutput" if debug else "Internal")).ap()
             for k, v in SCRATCH.items()}
    with ExitStack() as st:
        sems = [st.enter_context(nc.semaphore(f"sem{i}")) for i in range(100)]
        S = Sched(nc, sems)
        G.S = S
        G.ps = [st.enter_context(nc.psum_tensor(f"ps{i}", [128, 512], F32)) for i in range(8)]
        G.dres = {k: S.res("d_" + k) for k in list(SCRATCH) + ["x", "out", "w"]}
        G.halo = st.enter_context(nc.sbuf_tensor("halo", [128, FT, 2], F32))
        G.halor = S.res("halo")
        phases = []
        for l in range(n_layers):
            xsrc = (G.inp["x"], G.dres["x"]) if l == 0 else (G.scr["XO"], G.dres["XO"])
            last = (l == n_layers - 1)
            xdst = (G.out, G.dres["out"]) if last else (G.scr["XO"], G.dres["XO"])
            for hf in range(2):
                phases.append(("AB", lambda l=l, hf=hf, xsrc=xsrc: phase_AB(G, l, hf, xsrc)))
            phases.append(("C", lambda l=l: phase_C(G, l)))
            phases.append(("D", lambda l=l: phase_D(G, l)))
            for hf in range(2):
                phases.append(("E", lambda l=l, hf=hf, xsrc=xsrc: phase_E(G, l, hf, xsrc)))
            for hf in range(2):
                phases.append(("FG", lambda l=l, hf=hf: phase_FG(G, l, hf)))
            phases.append(("H", lambda l=l, xdst=xdst: phase_H(G, l, 0, xdst)))
        for name, fn in phases:
            fn()
            if stop_after is not None and name == stop_after:
                break
        with ExitStack() as ph:
            S.barrier()
            blk = ph.enter_context(nc.Block())
            S.emit(blk)
    return nc


def run_phase(G, fn):
    nc, S = G.nc, G.S
    with ExitStack() as ph:
        S.barrier()
        S.release_all()
        fn(ph)
        blk = ph.enter_context(nc.Block())
        S.emit(blk)


_UID = [0]


def sb(ph, G, name, shape, dt):
    _UID[0] += 1
    return ph.enter_context(G.nc.sbuf_tensor(f"t{_UID[0]}_{name}", shape, dt))


def weight_steps(G, wb, wbr, stg, stgr, kctr, wsrc, nkc, wres):
    S = G.S
    ncols = wsrc.shape[1]
    wv = wsrc.rearrange("(kc p) n -> p kc n", p=128)
    gsz = stg[0].shape[1]
    steps = []
    g0 = 0
    while g0 < nkc:
        g1 = min(nkc, g0 + gsz)

        def step(g0=g0, g1=g1):
            sl = kctr[0] % len(stg)
            kctr[0] += 1
            S.dma("sp", stg[sl][:, 0:g1 - g0, 0:ncols], wv[:, g0:g1, :], stgr[sl], reads=[wres], writes=[stgr[sl]])
            S.op("pool", lambda e, sl=sl: e.tensor_copy(out=wb[:, g0:g1, 0:ncols], in_=stg[sl][:, 0:g1 - g0, 0:ncols]),
                 reads=[stgr[sl]], writes=[wbr])
        steps.append(step)
        g0 = g1
    return steps


def weight_steps2(G, wb, wbr, stg, stgr, kctr, wsrc, nkc, wres, eng="act"):
    S = G.S
    ncols = wsrc.shape[1]
    wv = wsrc.rearrange("(kc p) n -> p kc n", p=128)
    gsz = stg[0].shape[1]
    steps = []
    g0 = 0
    while g0 < nkc:
        g1 = min(nkc, g0 + gsz)
        box = {}

        def dma_fn(g0=g0, g1=g1, box=box):
            sl = kctr[0] % len(stg)
            kctr[0] += 1
            box["sl"] = sl
            S.dma("sp", stg[sl][:, 0:g1 - g0, 0:ncols], wv[:, g0:g1, :], stgr[sl], reads=[wres], writes=[stgr[sl]])

        def cast_fn(g0=g0, g1=g1, box=box):
            sl = box["sl"]
            if eng == "act":
                S.op("act", lambda e: e.activation(out=wb[:, g0:g1, 0:ncols], in_=stg[sl][:, 0:g1 - g0, 0:ncols], func=AF.Copy),
                     reads=[stgr[sl]], writes=[wbr])
            else:
                S.op(eng, lambda e: e.tensor_copy(out=wb[:, g0:g1, 0:ncols], in_=stg[sl][:, 0:g1 - g0, 0:ncols]),
                     reads=[stgr[sl]], writes=[wbr])
        steps.append((dma_fn, cast_fn))
        g0 = g1
    return steps


def load_weight_block(G, wb, wbr, stg, stgr, kctr, wsrc, nkc, wres):
    for st_ in weight_steps(G, wb, wbr, stg, stgr, kctr, wsrc, nkc, wres):
        st_()


def phase_AB(G, l, hf, xsrc):
    def body(ph):
        nc, S = G.nc, G.S
        xap, xres = xsrc
        HT = sb(ph, G, "HT", [128, KC, 2048], BF16)
        HTr = S.res("HT")
        WB = [sb(ph, G, f"WB{i}", [128, KC, 512], BF16) for i in range(2)]
        WBr = [S.res(f"WB{i}") for i in range(2)]
        STG = [sb(ph, G, f"STG{i}", [128, 4, 512], F32) for i in range(4)]
        STGr = [S.res(f"STG{i}", dma=True) for i in range(4)]
        kctr = [0]
        xt = [sb(ph, G, f"xt{i}", [128, D], F32) for i in range(4)]
        xtr = [S.res(f"xt{i}", dma=True) for i in range(4)]
        hb = [sb(ph, G, f"hb{i}", [128, D], BF16) for i in range(2)]
        hbr = [S.res(f"hb{i}") for i in range(2)]
        g1b = sb(ph, G, "g1b", [128, D], F32)
        g1r = S.res("g1b", dma=True)
        junk = sb(ph, G, "junk", [128, D], BF16)
        junkr = S.res("junk")
        sm = sb(ph, G, "sm", [128, 64], F32)
        smr = [S.res(f"sm{i}") for i in range(8)]
        cs = {}
        csr = {}
        for k in ("ident", "kd"):
            cs[k] = sb(ph, G, "c_" + k, CONST_SHAPES[k][0], CONST_SHAPES[k][1])
            csr[k] = S.res("c_" + k, dma=True)
            S.dma("sp", cs[k][:], G.cst[k], csr[k], writes=[csr[k]])
        gq = sb(ph, G, "gq", [128, 2, 128], F32)
        gqr = S.res("gq", dma=True)
        S.dma("sp", gq[:, 0, :], G.inp["q_norm_g"][l].partition_broadcast(128), gqr, writes=[gqr])
        S.dma("sp", gq[:, 1, :], G.inp["k_norm_g"][l].partition_broadcast(128), gqr, writes=[gqr])
        S.op("dve", lambda e: e.tensor_scalar(out=gq[:, 0, :], in0=gq[:, 0, :], scalar1=float(128 ** -0.5), scalar2=None, op0=ALU.mult),
             reads=[gqr], writes=[gqr])
        S.dma("sp", g1b[:], G.inp["ln1_g"][l].partition_broadcast(128), g1r, writes=[g1r])
        ps = G.ps
        psr = [S.res(f"ps{i}") for i in range(8)]
        win = G.inp["w_in"][l]
        wres = G.dres["w"]
        load_weight_block(G, WB[0], WBr[0], STG, STGr, kctr, win[:, 0:512], KC, wres)
        def n_stats(tt):
            gt = hf * 16 + tt
            b = tt % 2
            xb = tt % 4
            if tt == 0:
                for t_ in range(2):
                    S.dma("sp", xt[t_][:], xap[(gt + t_) * 128:(gt + t_ + 1) * 128, :], xtr[t_], reads=[xres], writes=[xtr[t_]])
            if tt + 2 < 16:
                S.dma("sp", xt[(tt + 2) % 4][:], xap[(gt + 2) * 128:(gt + 3) * 128, :], xtr[(tt + 2) % 4], reads=[xres], writes=[xtr[(tt + 2) % 4]])
            c0 = (tt % 4) * 3
            ss, lnv, rstd = sm[:, c0:c0 + 1], sm[:, c0 + 1:c0 + 2], sm[:, c0 + 2:c0 + 3]
            r_s = smr[tt % 4]
            S.op("act", lambda e, xb=xb, ss=ss: e.activation(out=junk[:], in_=xt[xb][:], func=AF.Square, accum_out=ss),
                 reads=[xtr[xb]], writes=[junkr, r_s])
            S.op("act", lambda e, ss=ss, lnv=lnv: e.activation(out=lnv, in_=ss, func=AF.Ln, scale=1.0 / D, bias=EPS),
                 reads=[r_s], writes=[r_s])
            S.op("act", lambda e, lnv=lnv, rstd=rstd: e.activation(out=rstd, in_=lnv, func=AF.Exp, scale=-0.5),
                 reads=[r_s], writes=[r_s])

        def n_tail(tt):
            gt = hf * 16 + tt
            b = tt % 2
            xb = tt % 4
            c0 = (tt % 4) * 3
            ss, lnv, rstd = sm[:, c0:c0 + 1], sm[:, c0 + 1:c0 + 2], sm[:, c0 + 2:c0 + 3]
            r_s = smr[tt % 4]
            S.op("dve", lambda e, b=b, xb=xb, rstd=rstd: e.scalar_tensor_tensor(out=hb[b][:], in0=xt[xb][:], scalar=rstd, in1=g1b[:],
                                                                        op0=ALU.mult, op1=ALU.mult),
                 reads=[xtr[xb], r_s, g1r], writes=[hbr[b]])
            pa, pb = 3 + 2 * (tt % 2), 4 + 2 * (tt % 2)
            for kc in range(16):
                bank = pa if kc < 8 else pb
                pv = ps[bank][:].bitcast(BF16)
                S.op("pe", lambda e, b=b, kc=kc, pv=pv: e.transpose(out=pv[:, (kc % 8) * 128:(kc % 8 + 1) * 128],
                                                                     in_=hb[b][:, kc * 128:(kc + 1) * 128], identity=cs["ident"][:]),
                     reads=[hbr[b], csr["ident"]], writes=[psr[bank]])
            S.op("act", lambda e, tt=tt, pa=pa: e.activation(out=HT[:, 0:8, tt * 128:(tt + 1) * 128],
                                                             in_=ps[pa][:].bitcast(BF16).rearrange("p (k t) -> p k t", k=8), func=AF.Copy),
                 reads=[psr[pa]], writes=[HTr])
            S.op("dve", lambda e, tt=tt, pb=pb: e.tensor_copy(out=HT[:, 8:16, tt * 128:(tt + 1) * 128],
                                                              in_=ps[pb][:].bitcast(BF16).rearrange("p (k t) -> p k t", k=8)),
                 reads=[psr[pb]], writes=[HTr])
        n_stats(0)
        for tt in range(16):
            if tt + 1 < 16:
                n_stats(tt + 1)
            n_tail(tt)
        if DBG.get('stopA'):
            return
        NCB = DBG.get('ncb', 14)
        sb16 = [sb(ph, G, f"sb16_{i}", [128, 512], BF16) for i in range(3)]
        sb16r = [S.res(f"sb16_{i}") for i in range(3)]
        c32 = [sb(ph, G, f"c32_{i}", [128, 512], F32) for i in range(2)]
        c32r = [S.res(f"c32_{i}") for i in range(2)]
        tmo = [sb(ph, G, f"tmo{i}", [128, 512], F32) for i in range(3)]
        tmor = [S.res(f"tmo{i}", dma=True) for i in range(3)]
        fst = [sb(ph, G, f"fst{i}", [128, 4, 512], BF16) for i in range(2)]
        fstr = [S.res(f"fst{i}", dma=True) for i in range(2)]
        it = 0
        tmc = 0
        wsteps = []
        tailq = []
        for cb in range(14):
            if cb + 1 < 14:
                wsteps = weight_steps(G, WB[(cb + 1) % 2], WBr[(cb + 1) % 2], STG, STGr, kctr, win[:, (cb + 1) * 512:(cb + 2) * 512], KC, wres)
            w = WB[cb % 2]
            wr = WBr[cb % 2]
            for tt in range(16):
                gt = hf * 16 + tt
                par = gt % 2
                bank = it % 3
                it += 1
                if wsteps:
                    wsteps.pop(0)()
                for kc in range(16):
                    S.op("pe", lambda e, kc=kc, tt=tt, bank=bank, w=w: e.matmul(out=ps[bank][:], lhsT=HT[:, kc, tt * 128:(tt + 1) * 128],
                                                                                rhs=w[:, kc, :], start=(kc == 0), stop=(kc == 15)),
                         reads=[HTr, wr], writes=[psr[bank]])
                while len(tailq) > 1:
                    tailq.pop(0)()
                P = ps[bank]
                Pr = psr[bank]
                fm = cb in (0, 1, 2, 3, 8, 9, 10, 11)
                i2 = it % 2
                i3 = it % 3
                if cb in (0, 1, 2, 3):
                    S.op("act", lambda e, i3=i3, P=P: e.activation(out=sb16[i3][:], in_=P[:], func=AF.Copy),
                         reads=[Pr], writes=[sb16r[i3]])
                if cb in (2, 3):
                    ts = tmc % 3
                    tmc += 1
                    tv = tmo[ts][:].bitcast(BF16)[:, 0:512]
                    for hh in range(2):
                        h = (cb - 2) * 2 + hh
                        S.op("dve", lambda e, hh=hh, h=h, P=P, tv=tv, par=par: e.tensor_scalar(
                            out=tv[:, hh * 256:(hh + 1) * 256], in0=P[:, hh * 256:(hh + 1) * 256],
                            scalar1=cs["kd"][:, par, h:h + 1], scalar2=None, op0=ALU.mult),
                            reads=[Pr, csr["kd"], sb16r[i3]], writes=[tmor[ts]])
                    S.dma("sp", G.scr["RK"][gt * 128:(gt + 1) * 128, (cb - 2) * 512:(cb - 1) * 512], tv, tmor[ts],
                          reads=[tmor[ts]], writes=[G.dres["RK"]])
                if cb in (4, 5, 12, 13):
                    ts = tmc % 3
                    tmc += 1
                    tv = tmo[ts][:].bitcast(BF16)[:, 0:512]
                    if it % 2 == 0:
                        S.op("act", lambda e, P=P, tv=tv: e.activation(out=tv, in_=P[:], func=AF.Copy), reads=[Pr], writes=[tmor[ts]])
                    else:
                        S.op("dve", lambda e, P=P, tv=tv: e.tensor_copy(out=tv, in_=P[:]), reads=[Pr], writes=[tmor[ts]])
                    dst = G.scr["RV"] if cb < 8 else G.scr["MV"]
                    dr = G.dres["RV"] if cb < 8 else G.dres["MV"]
                    c0 = (cb - 4) * 512 if cb < 8 else (cb - 12) * 512
                    S.dma("sp", dst[gt * 128:(gt + 1) * 128, c0:c0 + 512], tv, tmor[ts], reads=[tmor[ts]], writes=[dr])
                if cb in (6, 7):
                    ts = tmc % 3
                    tmc += 1
                    S.op("act", lambda e, P=P, ts=ts: e.activation(out=tmo[ts][:], in_=P[:], func=AF.Silu), reads=[Pr], writes=[tmor[ts]])
                    S.dma("sp", G.scr["SG"][gt * 128:(gt + 1) * 128, (cb - 6) * 512:(cb - 5) * 512], tmo[ts][:], tmor[ts],
                          reads=[tmor[ts]], writes=[G.dres["SG"]])
                if cb in (8, 9, 10, 11):
                    gi = 0 if cb < 10 else 1
                    c0 = 12 + (it % 2) * 8
                    ss4, ln4, r4 = sm[:, c0:c0 + 4], sm[:, c0 + 4:c0 + 8], sm[:, c0 + 4:c0 + 8]
                    r_s = smr[4 + it % 2]
                    S.op("act", lambda e, i2=i2, P=P: e.activation(out=c32[i2][:], in_=P[:], func=AF.Copy), reads=[Pr], writes=[c32r[i2]])
                    for hh in range(4):
                        S.op("act", lambda e, i2=i2, hh=hh, ss4=ss4: e.activation(out=junk[:, hh * 128:(hh + 1) * 128], in_=c32[i2][:, hh * 128:(hh + 1) * 128],
                                                                                 func=AF.Square, accum_out=ss4[:, hh:hh + 1]),
                             reads=[c32r[i2]], writes=[junkr, r_s])
                    S.op("act", lambda e, ss4=ss4, ln4=ln4: e.activation(out=ln4, in_=ss4, func=AF.Ln, scale=1.0 / 128, bias=EPS),
                         reads=[r_s], writes=[r_s])
                    S.op("act", lambda e, ln4=ln4, r4=r4: e.activation(out=r4, in_=ln4, func=AF.Exp, scale=-0.5), reads=[r_s], writes=[r_s])
                    for hh in range(4):
                        S.op("dve", lambda e, i2=i2, i3=i3, hh=hh, r4=r4, gi=gi: e.scalar_tensor_tensor(
                            out=sb16[i3][:, hh * 128:(hh + 1) * 128], in0=c32[i2][:, hh * 128:(hh + 1) * 128], scalar=r4[:, hh:hh + 1],
                            in1=gq[:, gi, :], op0=ALU.mult, op1=ALU.mult),
                            reads=[c32r[i2], r_s, gqr], writes=[sb16r[i3]])
                if fm:
                    def fm_tail(cb=cb, tt=tt, i3=i3, it=it):
                        tb = 3 + (it % 4)
                        pv = ps[tb][:].bitcast(BF16)
                        for j in range(4):
                            S.op("pe", lambda e, j=j: e.transpose(out=pv[:, j * 128:(j + 1) * 128], in_=sb16[i3][:, j * 128:(j + 1) * 128],
                                                                  identity=cs["ident"][:]),
                                 reads=[sb16r[i3], csr["ident"]], writes=[psr[tb]])
                        fs = (tt // 4) % 2
                        src = pv[:, 0:512].rearrange("p (j t) -> p j t", j=4)
                        dstv = fst[fs][:, :, (tt % 4) * 128:(tt % 4 + 1) * 128]
                        if it % 2 == 0:
                            S.op("act", lambda e: e.activation(out=dstv, in_=src, func=AF.Copy), reads=[psr[tb]], writes=[fstr[fs]])
                        else:
                            S.op("dve", lambda e: e.tensor_copy(out=dstv, in_=src), reads=[psr[tb]], writes=[fstr[fs]])
                        if tt % 4 == 3:
                            nm, cbase = {0: ("RQT", 0), 1: ("RQT", 4), 2: ("RKT", 0), 3: ("RKT", 4), 8: ("MQT", 0), 9: ("MQT", 4),
                                         10: ("MKT", 0), 11: ("MKT", 4)}[cb]
                            tok0 = hf * 2048 + (tt // 4) * 512
                            S.dma("sp", G.scr[nm][cbase:cbase + 4, :, tok0:tok0 + 512].rearrange("c p t -> p c t"), fst[fs][:], fstr[fs],
                                  reads=[fstr[fs]], writes=[G.dres[nm]])
                    tailq.append(fm_tail)
        while tailq:
            tailq.pop(0)()
    run_phase(G, body)


def make_in_maps(inputs):
    cst = host_consts()
    maps = []
    for c in range(NCORES):
        m = {}
        for k, shp in IN_SHAPES.items():
            a = np.asarray(inputs[k])
            if k == "x":
                a = a[c]
            m[k] = np.ascontiguousarray(a.reshape(shp), dtype=np.float32)
        for k, v in cst.items():
            m["c_" + k] = v
        maps.append(m)
    return maps


_NC_CACHE = {}


def kernel(**inputs):
    if "nc" not in _NC_CACHE:
        _NC_CACHE["nc"] = build()
    nc = _NC_CACHE["nc"]
    res = run_bass_kernel_spmd(nc, make_in_maps(inputs), core_ids=list(range(NCORES)))
    return np.stack([np.asarray(res.results[c]["out"]).reshape(T, D) for c in range(NCORES)], axis=0).astype(np.float32)


def norm_to_HT(G, ph, S, HT, HTr, xap, xres, gain_ap, hf, ident, identr, psr):
    ps = G.ps
    xt = [sb(ph, G, f"xt{i}", [128, D], F32) for i in range(4)]
    xtr = [S.res(f"xt{i}", dma=True) for i in range(4)]
    hb = [sb(ph, G, f"hb{i}", [128, D], BF16) for i in range(2)]
    hbr = [S.res(f"hb{i}") for i in range(2)]
    g1b = sb(ph, G, "g1b", [128, D], F32)
    g1r = S.res("g1b", dma=True)
    junk = sb(ph, G, "junkn", [128, D], BF16)
    junkr = S.res("junkn")
    sm = sb(ph, G, "smn", [128, 16], F32)
    smr = [S.res(f"smn{i}") for i in range(4)]
    S.dma("sp", g1b[:], gain_ap.partition_broadcast(128), g1r, writes=[g1r])
    def n_stats(tt):
        gt = hf * 16 + tt
        b = tt % 2
        xb = tt % 4
        if tt == 0:
            for t_ in range(2):
                S.dma("sp", xt[t_][:], xap[(gt + t_) * 128:(gt + t_ + 1) * 128, :], xtr[t_], reads=[xres], writes=[xtr[t_]])
        if tt + 2 < 16:
            S.dma("sp", xt[(tt + 2) % 4][:], xap[(gt + 2) * 128:(gt + 3) * 128, :], xtr[(tt + 2) % 4], reads=[xres], writes=[xtr[(tt + 2) % 4]])
        c0 = (tt % 4) * 3
        ss, lnv, rstd = sm[:, c0:c0 + 1], sm[:, c0 + 1:c0 + 2], sm[:, c0 + 2:c0 + 3]
        r_s = smr[tt % 4]
        S.op("act", lambda e, xb=xb, ss=ss: e.activation(out=junk[:], in_=xt[xb][:], func=AF.Square, accum_out=ss),
             reads=[xtr[xb]], writes=[junkr, r_s])
        S.op("act", lambda e, ss=ss, lnv=lnv: e.activation(out=lnv, in_=ss, func=AF.Ln, scale=1.0 / D, bias=EPS), reads=[r_s], writes=[r_s])
        S.op("act", lambda e, lnv=lnv, rstd=rstd: e.activation(out=rstd, in_=lnv, func=AF.Exp, scale=-0.5), reads=[r_s], writes=[r_s])

    def n_tail(tt):
        gt = hf * 16 + tt
        b = tt % 2
        xb = tt % 4
        c0 = (tt % 4) * 3
        ss, lnv, rstd = sm[:, c0:c0 + 1], sm[:, c0 + 1:c0 + 2], sm[:, c0 + 2:c0 + 3]
        r_s = smr[tt % 4]
        S.op("dve", lambda e, b=b, xb=xb, rstd=rstd: e.scalar_tensor_tensor(out=hb[b][:], in0=xt[xb][:], scalar=rstd, in1=g1b[:],
                                                                    op0=ALU.mult, op1=ALU.mult),
             reads=[xtr[xb], r_s, g1r], writes=[hbr[b]])
        pa, pb = 3 + 2 * (tt % 2), 4 + 2 * (tt % 2)
        for kc in range(16):
            bank = pa if kc < 8 else pb
            pv = ps[bank][:].bitcast(BF16)
            S.op("pe", lambda e, b=b, kc=kc, pv=pv: e.transpose(out=pv[:, (kc % 8) * 128:(kc % 8 + 1) * 128],
                                                                 in_=hb[b][:, kc * 128:(kc + 1) * 128], identity=ident[:]),
                 reads=[hbr[b], identr], writes=[psr[bank]])
        S.op("act", lambda e, tt=tt, pa=pa: e.activation(out=HT[:, 0:8, tt * 128:(tt + 1) * 128],
                                                         in_=ps[pa][:].bitcast(BF16).rearrange("p (k t) -> p k t", k=8), func=AF.Copy),
             reads=[psr[pa]], writes=[HTr])
        S.op("dve", lambda e, tt=tt, pb=pb: e.tensor_copy(out=HT[:, 8:16, tt * 128:(tt + 1) * 128],
                                                          in_=ps[pb][:].bitcast(BF16).rearrange("p (k t) -> p k t", k=8)),
             reads=[psr[pb]], writes=[HTr])
    n_stats(0)
    for tt in range(16):
        if tt + 1 < 16:
            n_stats(tt + 1)
        n_tail(tt)


def phase_E(G, l, hf, xsrc):
    def body(ph):
        S = G.S
        ps = G.ps
        xap, xres = xsrc
        HT = sb(ph, G, "YT", [128, KC, 2048], BF16)
        HTq = [S.res(f"YTq{i}", dma=True) for i in range(4)]
        WB = [sb(ph, G, f"WB{i}", [128, KC, 512], BF16) for i in range(2)]
        WBr = [S.res(f"WB{i}") for i in range(2)]
        STG = [sb(ph, G, f"STG{i}", [128, 4, 512], F32) for i in range(4)]
        STGr = [S.res(f"STG{i}", dma=True) for i in range(4)]
        kctr = [0]
        xa = [sb(ph, G, f"xa{i}", [128, 512], F32) for i in range(4)]
        xar = [S.res(f"xa{i}", dma=True) for i in range(4)]
        psr = [S.res(f"ps{i}") for i in range(8)]
        w_out = G.inp["w_out"][l]
        load_weight_block(G, WB[0], WBr[0], STG, STGr, kctr, w_out[:, 0:512], KC, G.dres["w"])
        for qi in range(4):
            S.dma("sp", HT[:, :, qi * 512:(qi + 1) * 512], G.scr["YTD"][:, :, hf * 2048 + qi * 512:hf * 2048 + (qi + 1) * 512].rearrange("c p t -> p c t"),
                  HTq[qi], reads=[G.dres["YTD"]], writes=[HTq[qi]])
        iters = [(cb, tt) for cb in range(4) for tt in range(16)]

        def ldx(i):
            cb, tt = iters[i]
            gt = hf * 16 + tt
            S.dma("sp", xa[i % 4][:], xap[gt * 128:(gt + 1) * 128, cb * 512:(cb + 1) * 512], xar[i % 4], reads=[xres], writes=[xar[i % 4]])
        ldx(0)
        ldx(1)
        for it, (cb, tt) in enumerate(iters):
            if tt == 0:
                wsteps = weight_steps(G, WB[(cb + 1) % 2], WBr[(cb + 1) % 2], STG, STGr, kctr, w_out[:, (cb + 1) * 512:(cb + 2) * 512], KC, G.dres["w"]) if cb + 1 < 4 else []
            if wsteps:
                wsteps.pop(0)()
            if it + 2 < len(iters):
                ldx(it + 2)
            w, wr = WB[cb % 2], WBr[cb % 2]
            gt = hf * 16 + tt
            bank = it % 4
            xs = it % 4
            for kc in range(16):
                S.op("pe", lambda e, kc=kc, tt=tt, bank=bank, w=w: e.matmul(out=ps[bank][:], lhsT=HT[:, kc, tt * 128:(tt + 1) * 128],
                                                                            rhs=w[:, kc, :], start=(kc == 0), stop=(kc == 15)),
                     reads=[HTq[tt // 4], wr], writes=[psr[bank]])
            S.op("dve", lambda e, xs=xs, bank=bank: e.tensor_tensor(out=xa[xs][:], in0=ps[bank][:], in1=xa[xs][:], op=ALU.add),
                 reads=[psr[bank], xar[xs]], writes=[xar[xs]])
            S.dma("sp", G.scr["XR"][gt * 128:(gt + 1) * 128, cb * 512:(cb + 1) * 512], xa[xs][:], xar[xs],
                  reads=[xar[xs]], writes=[G.dres["XR"]])
    run_phase(G, body)


def phase_FG(G, l, hf):
    def body(ph):
        S = G.S
        ps = G.ps
        HT = sb(ph, G, "HT", [128, KC, 2048], BF16)
        HTr = S.res("HT")
        psr = [S.res(f"ps{i}") for i in range(8)]
        ident = sb(ph, G, "ident", [128, 128], BF16)
        identr = S.res("ident", dma=True)
        S.dma("sp", ident[:], G.cst["ident"], identr, writes=[identr])
        with ExitStack() as sub:
            norm_to_HT(G, sub, S, HT, HTr, G.scr["XR"], G.dres["XR"], G.inp["ln2_g"][l], hf, ident, identr, psr)
            S.barrier()
        identf = sb(ph, G, "identf", [128, 128], F32)
        identfr = S.res("identf", dma=True)
        S.dma("sp", identf[:], G.cst["identf"], identfr, writes=[identfr])
        cwT = sb(ph, G, "cwT", [FT, 4, 128], F32)
        cwTr = S.res("cwT", dma=True)
        S.dma("sp", cwT[:, 0:3, :], G.inp["conv_w"][l].rearrange("j (f p) -> f j p", p=128), cwTr, writes=[cwTr])
        S.dma("sp", cwT[:, 3, :], G.inp["conv_b"][l].rearrange("(f p) -> f p", p=128), cwTr, writes=[cwTr])
        cw = sb(ph, G, "cw", [128, 4, FT], F32)
        cwr = S.res("cw")
        for j in range(4):
            S.op("pe", lambda e, j=j: e.transpose(out=ps[7][:, j * FT:(j + 1) * FT], in_=cwT[:, j, :], identity=identf[0:FT, 0:FT]),
                 reads=[cwTr, identfr], writes=[psr[7]])
        S.op("dve", lambda e: e.tensor_copy(out=cw[:].rearrange("p j f -> p (j f)"), in_=ps[7][:, 0:4 * FT]), reads=[psr[7]], writes=[cwr])
        WG = [sb(ph, G, f"WG{i}", [128, KC, 512], BF16) for i in range(2)]
        WGr = [S.res(f"WG{i}") for i in range(2)]
        WU = [sb(ph, G, f"WU{i}", [128, KC, 512], BF16) for i in range(2)]
        WUr = [S.res(f"WU{i}") for i in range(2)]
        STG = [sb(ph, G, f"STG{i}", [128, 4, 512], F32) for i in range(4)]
        STGr = [S.res(f"STG{i}", dma=True) for i in range(4)]
        kctr = [0]
        asb = [sb(ph, G, f"asb{i}", [128, 2 + 2048], F32) for i in range(2)]
        asbr = [S.res(f"asb{i}") for i in range(2)]
        cbuf = [sb(ph, G, f"cbuf{i}", [128, 512], F32) for i in range(2)]
        cbufr = [S.res(f"cbuf{i}") for i in range(2)]
        gsb = [sb(ph, G, f"gsb{i}", [128, 512], BF16) for i in range(3)]
        gsbr = [S.res(f"gsb{i}", dma=True) for i in range(3)]
        wg, wu = G.inp["w_gate"][l], G.inp["w_up"][l]

        def ldw(fb):
            return (weight_steps2(G, WG[fb % 2], WGr[fb % 2], STG, STGr, kctr, wg[:, fb * 512:(fb + 1) * 512], KC, G.dres["w"]) +
                    weight_steps2(G, WU[fb % 2], WUr[fb % 2], STG, STGr, kctr, wu[:, fb * 512:(fb + 1) * 512], KC, G.dres["w"]))
        pend_cast = []
        for d_, c_ in ldw(0):
            d_()
            c_()
        it = 0
        wsteps = []
        for fb in range(11):
            if fb + 1 < 11:
                wsteps = ldw(fb + 1)
            for f4 in range(4):
                ft = fb * 4 + f4
                a = asb[ft % 2]
                ar = asbr[ft % 2]
                if hf == 0:
                    S.op("dve", lambda e, a=a: e.memset(a[:, 0:2], 0.0), writes=[ar])
                else:
                    S.op("dve", lambda e, a=a, ft=ft: e.tensor_copy(out=a[:, 0:2], in_=G.halo[:, ft, :]), reads=[G.halor], writes=[ar])
                for tg in range(4):
                    ba, bu = (it % 2) * 2, (it % 2) * 2 + 1
                    cbi = it % 2
                    gs = it % 3
                    it += 1
                    while pend_cast:
                        pend_cast.pop(0)()
                    if wsteps:
                        d_, c_ = wsteps.pop(0)
                        d_()
                        pend_cast.append(c_)
                    for kc in range(16):
                        S.op("pe", lambda e, kc=kc, f4=f4, tg=tg, ba=ba, fb=fb: e.matmul(
                            out=ps[ba][:], lhsT=WG[fb % 2][:, kc, f4 * 128:(f4 + 1) * 128], rhs=HT[:, kc, tg * 512:(tg + 1) * 512],
                            start=(kc == 0), stop=(kc == 15)), reads=[HTr, WGr[fb % 2]], writes=[psr[ba]])
                    for kc in range(16):
                        S.op("pe", lambda e, kc=kc, f4=f4, tg=tg, bu=bu, fb=fb: e.matmul(
                            out=ps[bu][:], lhsT=WU[fb % 2][:, kc, f4 * 128:(f4 + 1) * 128], rhs=HT[:, kc, tg * 512:(tg + 1) * 512],
                            start=(kc == 0), stop=(kc == 15)), reads=[HTr, WUr[fb % 2]], writes=[psr[bu]])
                    t0 = 2 + tg * 512
                    S.op("act", lambda e, a=a, t0=t0, ba=ba: e.activation(out=a[:, t0:t0 + 512], in_=ps[ba][:], func=AF.Copy),
                         reads=[psr[ba]], writes=[ar])
                    c = cbuf[cbi]
                    cr = cbufr[cbi]
                    S.op("dve", lambda e, a=a, t0=t0, c=c, ft=ft: e.tensor_scalar(out=c[:], in0=a[:, t0:t0 + 512], scalar1=cw[:, 2, ft:ft + 1],
                                                                              scalar2=cw[:, 3, ft:ft + 1], op0=ALU.mult, op1=ALU.add),
                         reads=[ar, cwr], writes=[cr])
                    S.op("dve", lambda e, a=a, t0=t0, c=c, ft=ft: e.scalar_tensor_tensor(out=c[:], in0=a[:, t0 - 1:t0 + 511], scalar=cw[:, 1, ft:ft + 1],
                                                                                     in1=c[:], op0=ALU.mult, op1=ALU.add),
                         reads=[ar, cwr, cr], writes=[cr])
                    S.op("dve", lambda e, a=a, t0=t0, c=c, ft=ft: e.scalar_tensor_tensor(out=c[:], in0=a[:, t0 - 2:t0 + 510], scalar=cw[:, 0, ft:ft + 1],
                                                                                     in1=c[:], op0=ALU.mult, op1=ALU.add),
                         reads=[ar, cwr, cr], writes=[cr])
                    S.op("act", lambda e, c=c: e.activation(out=c[:], in_=c[:], func=AF.Silu), reads=[cr], writes=[cr])
                    S.op("dve", lambda e, c=c, gs=gs, bu=bu: e.tensor_tensor(out=gsb[gs][:], in0=ps[bu][:], in1=c[:], op=ALU.mult),
                         reads=[psr[bu], cr], writes=[gsbr[gs]])
                    tok0 = hf * 2048 + tg * 512
                    S.dma("sp", G.scr["GT"][ft, :, tok0:tok0 + 512], gsb[gs][:], gsbr[gs], reads=[gsbr[gs]], writes=[G.dres["GT"]])
                if hf == 0:
                    S.op("dve", lambda e, a=a, ft=ft: e.tensor_copy(out=G.halo[:, ft, :], in_=a[:, 2048:2050]), reads=[ar], writes=[G.halor])
    run_phase(G, body)


def phase_H(G, l, hf, xdst):
    def body(ph):
        S = G.S
        ps = G.ps
        oap, ores = xdst
        WD = [sb(ph, G, f"WD{i}", [128, FT, 512], BF16) for i in range(2)]
        WDr = [S.res(f"WD{i}") for i in range(2)]
        STG = [sb(ph, G, f"STG{i}", [128, 4, 512], F32) for i in range(2)]
        STGr = [S.res(f"STG{i}", dma=True) for i in range(2)]
        kctr = [0]
        GB = [sb(ph, G, f"GB{i}", [128, FT, 512], BF16) for i in range(2)]
        GBr = [S.res(f"GB{i}", dma=True) for i in range(2)]
        xa = [sb(ph, G, f"xa{i}", [128, 512], F32) for i in range(4)]
        xar = [S.res(f"xa{i}", dma=True) for i in range(4)]
        psr = [S.res(f"ps{i}") for i in range(8)]
        wd = G.inp["w_down"][l]
        load_weight_block(G, WD[0], WDr[0], STG, STGr, kctr, wd[:, 0:512], FT, G.dres["w"])
        gi = 0

        def ldg(gidx):
            tg = gidx % 8
            tok0 = tg * 512
            S.dma("sp", GB[gidx % 2][:], G.scr["GT"][:, :, tok0:tok0 + 512].rearrange("f p t -> p f t"), GBr[gidx % 2],
                  reads=[G.dres["GT"]], writes=[GBr[gidx % 2]])
        ldg(0)
        iters = [(cb, tg, t4) for cb in range(4) for tg in range(8) for t4 in range(4)]

        def ldx(i):
            cb, tg, t4 = iters[i]
            gt = tg * 4 + t4
            S.dma("sp", xa[i % 4][:], G.scr["XR"][gt * 128:(gt + 1) * 128, cb * 512:(cb + 1) * 512], xar[i % 4],
                  reads=[G.dres["XR"]], writes=[xar[i % 4]])
        ldx(0)
        ldx(1)
        for it, (cb, tg, t4) in enumerate(iters):
            if tg == 0 and t4 == 0:
                wsteps = weight_steps(G, WD[(cb + 1) % 2], WDr[(cb + 1) % 2], STG, STGr, kctr, wd[:, (cb + 1) * 512:(cb + 2) * 512], FT, G.dres["w"]) if cb + 1 < 4 else []
            if wsteps:
                wsteps.pop(0)()
            if t4 == 0:
                if gi + 1 < 32:
                    ldg(gi + 1)
                gb, gbr = GB[gi % 2], GBr[gi % 2]
                gi += 1
            if it + 2 < len(iters):
                ldx(it + 2)
            w, wr = WD[cb % 2], WDr[cb % 2]
            gt = tg * 4 + t4
            bank = it % 4
            xs = it % 4
            for fc in range(FT):
                S.op("pe", lambda e, fc=fc, t4=t4, bank=bank, w=w, gb=gb: e.matmul(out=ps[bank][:], lhsT=gb[:, fc, t4 * 128:(t4 + 1) * 128],
                                                                                rhs=w[:, fc, :], start=(fc == 0), stop=(fc == FT - 1)),
                     reads=[gbr, wr], writes=[psr[bank]])
            S.op("dve", lambda e, xs=xs, bank=bank: e.tensor_tensor(out=xa[xs][:], in0=ps[bank][:], in1=xa[xs][:], op=ALU.add),
                 reads=[psr[bank], xar[xs]], writes=[xar[xs]])
            S.dma("sp", oap[gt * 128:(gt + 1) * 128, cb * 512:(cb + 1) * 512], xa[xs][:], xar[xs], reads=[xar[xs]], writes=[ores])
    run_phase(G, body)


def phase_C(G, l):
    def body(ph):
        S = G.S
        ps = G.ps
        psr = [S.res(f"ps{i}") for i in range(8)]
        cs, csr = {}, {}
        for k in ("ident", "dec", "qd"):
            cs[k] = sb(ph, G, "c_" + k, CONST_SHAPES[k][0], CONST_SHAPES[k][1])
            csr[k] = S.res("c_" + k, dma=True)
            S.dma("sp", cs[k][:], G.cst[k], csr[k], writes=[csr[k]])
        gr = sb(ph, G, "gr", [128, 1024], F32)
        grr = S.res("gr", dma=True)
        S.dma("sp", gr[:], G.inp["ret_norm_g"][l].partition_broadcast(128), grr, writes=[grr])
        qT = [sb(ph, G, f"qT{i}", [128, 2, 1024], BF16) for i in range(2)]
        kT = [sb(ph, G, f"kT{i}", [128, 2, 1024], BF16) for i in range(2)]
        kk = [sb(ph, G, f"kk{i}", [128, 8, 256], BF16) for i in range(2)]
        vv = [sb(ph, G, f"vv{i}", [128, 8, 256], BF16) for i in range(2)]
        sg = [sb(ph, G, f"sg{i}", [128, 8, 256], F32) for i in range(2)]
        qTr = [S.res(f"qT{i}", dma=True) for i in range(2)]
        kTr = [S.res(f"kT{i}", dma=True) for i in range(2)]
        kkr = [S.res(f"kk{i}", dma=True) for i in range(2)]
        vvr = [S.res(f"vv{i}", dma=True) for i in range(2)]
        sgr = [S.res(f"sg{i}", dma=True) for i in range(2)]
        st32 = sb(ph, G, "st32", [128, 512], F32)
        st32r = S.res("st32")
        stb = [sb(ph, G, f"stb{i}", [128, 2, 256], BF16) for i in range(2)]
        stbr = [S.res(f"stb{i}") for i in range(2)]
        PT = [sb(ph, G, f"PT{i}", [128, 2, 256], BF16) for i in range(2)]
        PTr = [S.res(f"PT{i}") for i in range(2)]
        ytmp = [sb(ph, G, f"ytmp{i}", [128, 512], F32) for i in range(2)]
        ytmpr = [S.res(f"ytmp{i}") for i in range(2)]
        yy = [sb(ph, G, f"yy{i}", [128, 512], F32) for i in range(2)]
        yyr = [S.res(f"yy{i}") for i in range(2)]
        t2 = [sb(ph, G, f"t2{i}", [128, 512], F32) for i in range(2)]
        t2r = [S.res(f"t2{i}") for i in range(2)]
        zz = [sb(ph, G, f"zz{i}", [128, 512], BF16) for i in range(2)]
        zzr = [S.res(f"zz{i}") for i in range(2)]
        junk = sb(ph, G, "junkc", [128, 256], BF16)
        junkr = S.res("junkc")
        sm = sb(ph, G, "smc", [128, 16], F32)
        smr = [S.res(f"smc{i}") for i in range(2)]
        yts = [sb(ph, G, f"yts{i}", [128, 2, 1024], BF16) for i in range(2)]
        ytsr = [S.res(f"yts{i}", dma=True) for i in range(2)]

        def load_group(h, g, s):
            t0 = g * 1024
            S.dma("sp", qT[s][:], G.scr["RQT"][h * 2:h * 2 + 2, :, t0:t0 + 1024].rearrange("c p t -> p c t"), qTr[s], reads=[G.dres["RQT"]], writes=[qTr[s]])
            S.dma("sp", kT[s][:], G.scr["RKT"][h * 2:h * 2 + 2, :, t0:t0 + 1024].rearrange("c p t -> p c t"), kTr[s], reads=[G.dres["RKT"]], writes=[kTr[s]])
            S.dma("sp", kk[s][:], G.scr["RK"][t0:t0 + 1024, h * 256:(h + 1) * 256].rearrange("(t p) c -> p t c", p=128), kkr[s], reads=[G.dres["RK"]], writes=[kkr[s]])
            S.dma("sp", vv[s][:], G.scr["RV"][t0:t0 + 1024, h * 256:(h + 1) * 256].rearrange("(t p) c -> p t c", p=128), vvr[s], reads=[G.dres["RV"]], writes=[vvr[s]])
            S.dma("sp", sg[s][:], G.scr["SG"][t0:t0 + 1024, h * 256:(h + 1) * 256].rearrange("(t p) c -> p t c", p=128), sgr[s], reads=[G.dres["SG"]], writes=[sgr[s]])
        groups = [(h, g) for h in range(4) for g in range(4)]
        chunks = [(h, n) for h in range(4) for n in range(16)]
        stb3 = stb + [sb(ph, G, "stb2", [128, 2, 256], BF16)]
        stb3r = stbr + [S.res("stb2")]

        def stage1(c):
            h, n = chunks[c]
            s, n4, i2 = (c // 4) % 2, n % 4, c % 2
            lt0 = n4 * 2
            for si in range(2):
                for dc in range(2):
                    S.op("pe", lambda e, si=si, dc=dc: e.matmul(
                        out=ps[i2][:, si * 256:(si + 1) * 256], lhsT=kT[s][:, dc, (lt0 + si) * 128:(lt0 + si + 1) * 128],
                        rhs=qT[s][:, dc, n4 * 256:(n4 + 1) * 256], start=(dc == 0), stop=(dc == 1)),
                        reads=[kTr[s], qTr[s]], writes=[psr[i2]])
            S.op("dve", lambda e: e.tensor_tensor(out=PT[i2][:].rearrange("p a c -> p (a c)"), in0=ps[i2][:],
                                                  in1=cs["dec"][:, h].rearrange("p a c -> p (a c)"), op=ALU.mult),
                 reads=[psr[i2], csr["dec"]], writes=[PTr[i2]])
            if n < 15:
                cdec = float(np.float32(GAMMA[h]) ** 256)
                for dc in range(2):
                    for si in range(2):
                        S.op("pe", lambda e, dc=dc, si=si: e.matmul(
                            out=ps[4][:, dc * 256:(dc + 1) * 256], lhsT=kk[s][:, lt0 + si, dc * 128:(dc + 1) * 128], rhs=vv[s][:, lt0 + si, :],
                            start=(si == 0), stop=(si == 1)), reads=[kkr[s], vvr[s]], writes=[psr[4]])
                if n == 0:
                    S.op("dve", lambda e: e.tensor_copy(out=st32[:], in_=ps[4][:]), reads=[psr[4]], writes=[st32r])
                else:
                    S.op("dve", lambda e: e.scalar_tensor_tensor(out=st32[:], in0=st32[:], scalar=cdec, in1=ps[4][:], op0=ALU.mult, op1=ALU.add),
                         reads=[psr[4], st32r], writes=[st32r])
                nx = (c + 1) % 3
                S.op("pool", lambda e: e.tensor_copy(out=stb3[nx][:].rearrange("p a c -> p (a c)"), in_=st32[:]), reads=[st32r], writes=[stb3r[nx]])

        def stage2(c):
            h, n = chunks[c]
            s, n4, i2 = (c // 4) % 2, n % 4, c % 2
            lt0 = n4 * 2
            cur = c % 3
            for ci in range(2):
                for si in range(ci + 1):
                    S.op("pe", lambda e, ci=ci, si=si: e.matmul(
                        out=ps[2][:, ci * 256:(ci + 1) * 256], lhsT=PT[i2][:, si, ci * 128:(ci + 1) * 128], rhs=vv[s][:, lt0 + si, :],
                        start=(si == 0), stop=(si == ci)), reads=[PTr[i2], vvr[s]], writes=[psr[2]])
            if n > 0:
                for ci in range(2):
                    for dc in range(2):
                        S.op("pe", lambda e, ci=ci, dc=dc: e.matmul(
                            out=ps[3][:, ci * 256:(ci + 1) * 256], lhsT=qT[s][:, dc, n4 * 256 + ci * 128:n4 * 256 + (ci + 1) * 128],
                            rhs=stb3[cur][:, dc, :], start=(dc == 0), stop=(dc == 1)), reads=[qTr[s], stb3r[cur]], writes=[psr[3]])
                for ci in range(2):
                    S.op("act", lambda e, ci=ci: e.activation(out=ytmp[i2][:, ci * 256:(ci + 1) * 256], in_=ps[3][:, ci * 256:(ci + 1) * 256],
                                                              func=AF.Copy, scale=cs["qd"][:, ci, h:h + 1]),
                         reads=[psr[3], csr["qd"]], writes=[ytmpr[i2]])
                S.op("dve", lambda e: e.tensor_tensor(out=yy[i2][:], in0=ps[2][:], in1=ytmp[i2][:], op=ALU.add),
                     reads=[psr[2], ytmpr[i2]], writes=[yyr[i2]])
            else:
                S.op("dve", lambda e: e.tensor_copy(out=yy[i2][:], in_=ps[2][:]), reads=[psr[2]], writes=[yyr[i2]])
            c0 = i2 * 4
            for ci in range(2):
                S.op("act", lambda e, ci=ci: e.activation(out=junk[:], in_=yy[i2][:, ci * 256:(ci + 1) * 256], func=AF.Square,
                                                          accum_out=sm[:, c0 + ci:c0 + ci + 1]),
                     reads=[yyr[i2]], writes=[junkr, smr[i2]])
            S.op("act", lambda e: e.activation(out=sm[:, c0 + 2:c0 + 4], in_=sm[:, c0:c0 + 2], func=AF.Ln, scale=1.0 / 256, bias=EPS),
                 reads=[smr[i2]], writes=[smr[i2]])
            S.op("act", lambda e: e.activation(out=sm[:, c0 + 2:c0 + 4], in_=sm[:, c0 + 2:c0 + 4], func=AF.Exp, scale=-0.5),
                 reads=[smr[i2]], writes=[smr[i2]])
            for ci in range(2):
                S.op("pool", lambda e, ci=ci: e.tensor_tensor(out=t2[i2][:, ci * 256:(ci + 1) * 256], in0=sg[s][:, lt0 + ci, :],
                                                              in1=gr[:, h * 256:(h + 1) * 256], op=ALU.mult),
                     reads=[sgr[s], grr], writes=[t2r[i2]])
            for ci in range(2):
                S.op("dve", lambda e, ci=ci: e.scalar_tensor_tensor(out=zz[i2][:, ci * 256:(ci + 1) * 256], in0=yy[i2][:, ci * 256:(ci + 1) * 256],
                                                                    scalar=sm[:, c0 + 2 + ci:c0 + 3 + ci], in1=t2[i2][:, ci * 256:(ci + 1) * 256],
                                                                    op0=ALU.mult, op1=ALU.mult),
                     reads=[yyr[i2], smr[i2], t2r[i2]], writes=[zzr[i2]])

        def stage3(c):
            h, n = chunks[c]
            s, n4, i2 = (c // 4) % 2, n % 4, c % 2
            trb = 5 + i2
            pv = ps[trb][:].bitcast(BF16)
            for ec in range(2):
                for ci in range(2):
                    S.op("pe", lambda e, ec=ec, ci=ci: e.transpose(out=pv[:, ec * 256 + ci * 128:ec * 256 + (ci + 1) * 128],
                                                                   in_=zz[i2][:, ci * 256 + ec * 128:ci * 256 + (ec + 1) * 128], identity=cs["ident"][:]),
                         reads=[zzr[i2], csr["ident"]], writes=[psr[trb]])
            S.op("act", lambda e: e.activation(out=yts[s][:, :, n4 * 256:(n4 + 1) * 256], in_=pv[:, 0:512].rearrange("p (a t) -> p a t", a=2),
                                               func=AF.Copy), reads=[psr[trb]], writes=[ytsr[s]])
            if n4 == 3:
                g = n // 4
                S.dma("sp", G.scr["YTD"][h * 2:h * 2 + 2, :, g * 1024:(g + 1) * 1024].rearrange("c p t -> p c t"), yts[s][:], ytsr[s],
                      reads=[ytsr[s]], writes=[G.dres["YTD"]])
        load_group(groups[0][0], groups[0][1], 0)
        load_group(groups[1][0], groups[1][1], 1)
        NCH = len(chunks)
        stage1(0)
        for c in range(NCH):
            if c + 1 < NCH:
                stage1(c + 1)
            stage2(c)
            if c % 4 == 3 and c // 4 + 2 < len(groups):
                gg = c // 4 + 2
                load_group(groups[gg][0], groups[gg][1], gg % 2)
            if c >= 1:
                stage3(c - 1)
        stage3(NCH - 1)
    run_phase(G, body)


def phase_D(G, l):
    def body(ph):
        S = G.S
        ps = G.ps
        psr = [S.res(f"ps{i}") for i in range(8)]
        cs, csr = {}, {}
        for k in ("ident", "ones", "bl", "tri", "negb", "validf", "own1h"):
            cs[k] = sb(ph, G, "c_" + k, CONST_SHAPES[k][0], CONST_SHAPES[k][1])
            csr[k] = S.res("c_" + k, dma=True)
            S.dma("sp", cs[k][:], G.cst[k], csr[k], writes=[csr[k]])
        qT = [sb(ph, G, f"mq{i}", [128, T], BF16) for i in range(2)]
        kT = [sb(ph, G, f"mk{i}", [128, T], BF16) for i in range(2)]
        vv = [sb(ph, G, f"mv{i}", [128, 32, 128], BF16) for i in range(2)]
        brhs = [sb(ph, G, f"brhs{i}", [128, T], BF16) for i in range(2)]
        qTr = [S.res(f"mq{i}", dma=True) for i in range(2)]
        kTr = [S.res(f"mk{i}", dma=True) for i in range(2)]
        vvr = [S.res(f"mv{i}", dma=True) for i in range(2)]
        brr = [S.res(f"brhs{i}", dma=True) for i in range(2)]
        km32 = sb(ph, G, "km32", [128, 16], F32)
        kmb = sb(ph, G, "kmb", [128, 2, 16], BF16)
        kmr = S.res("km")
        gsel = sb(ph, G, "gsel", [128, 512], F32)
        sel = sb(ph, G, "sel", [128, 512], F32)
        top8 = sb(ph, G, "top8", [128, 256], F32)
        maskb = sb(ph, G, "maskb", [128, 512], BF16)
        selr = S.res("sel")
        PT = [sb(ph, G, f"PTm{i}", [128, 512], BF16) for i in range(3)]
        PTr = [S.res(f"PTm{i}") for i in range(3)]
        rden = sb(ph, G, "rden", [128, 512], F32)
        rdenr = S.res("rden")
        yo = [sb(ph, G, f"yo{i}", [128, 512], BF16) for i in range(2)]
        yor = [S.res(f"yo{i}", dma=True) for i in range(2)]
        for i in range(2):
            S.op("pool", lambda e, i=i: e.memset(brhs[i][:], 0.0), writes=[brr[i]])

        def load_head(h, s):
            S.dma("sp", qT[s][:], G.scr["MQT"][h], qTr[s], reads=[G.dres["MQT"]], writes=[qTr[s]])
            S.dma("sp", kT[s][:], G.scr["MKT"][h], kTr[s], reads=[G.dres["MKT"]], writes=[kTr[s]])
            S.dma("sp", vv[s][:], G.scr["MV"][:, h * 128:(h + 1) * 128].rearrange("(t p) d -> p t d", p=128), vvr[s], reads=[G.dres["MV"]], writes=[vvr[s]])
            S.dma("sp", brhs[s][32:36, :], G.cst["brc"][h], brr[s], writes=[brr[s]])
        pv7 = ps[7][:].bitcast(BF16)

        def ms_a(h):
            s = h % 2
            S.op("dve", lambda e: e.tensor_reduce(out=km32[:], in_=kT[s][:].rearrange("p (b t) -> p b t", b=16), axis=AX.X, op=ALU.add),
                 reads=[kTr[s]], writes=[kmr])
            S.op("dve", lambda e: e.tensor_scalar(out=km32[:], in0=km32[:], scalar1=1.0 / 256, scalar2=None, op0=ALU.mult), reads=[kmr], writes=[kmr])
            S.op("dve", lambda e: e.tensor_copy(out=kmb[:, 0, :], in_=km32[:]), reads=[kmr], writes=[kmr])
            S.op("dve", lambda e: e.tensor_tensor(out=kmb[:, 1, :], in0=km32[:], in1=kmb[:, 0, :], op=ALU.subtract), reads=[kmr], writes=[kmr])

        def ms_b(h):
            s = h % 2
            for qt in range(32):
                for a_ in range(2):
                    S.op("pe", lambda e, qt=qt, a_=a_: e.matmul(out=ps[6][:, qt * 16:(qt + 1) * 16], lhsT=qT[s][:, qt * 128:(qt + 1) * 128],
                                                               rhs=kmb[:, a_, :], start=(a_ == 0), stop=(a_ == 1)),
                         reads=[qTr[s], kmr], writes=[psr[6]])
            S.op("dve", lambda e: e.tensor_tensor(out=gsel[:], in0=ps[6][:], in1=cs["negb"][:].rearrange("p a b -> p (a b)"), op=ALU.add),
                 reads=[psr[6], csr["negb"]], writes=[selr])
            for qt in range(32):
                S.op("dve", lambda e, qt=qt: e.max(out=top8[:, qt * 8:(qt + 1) * 8], in_=gsel[:, qt * 16:(qt + 1) * 16]), reads=[selr], writes=[selr])
            for qt in range(32):
                S.op("dve", lambda e, qt=qt: e.tensor_scalar(out=sel[:, qt * 16:(qt + 1) * 16], in0=gsel[:, qt * 16:(qt + 1) * 16],
                                                             scalar1=top8[:, qt * 8 + 2:qt * 8 + 3], scalar2=None, op0=ALU.is_ge),
                     reads=[selr], writes=[selr])
            S.op("dve", lambda e: e.tensor_tensor(out=sel[:], in0=sel[:], in1=cs["validf"][:].rearrange("p a b -> p (a b)"), op=ALU.mult),
                 reads=[selr, csr["validf"]], writes=[selr])
            S.op("dve", lambda e: e.tensor_tensor(out=sel[:], in0=sel[:], in1=cs["own1h"][:].rearrange("p a b -> p (a b)"), op=ALU.add),
                 reads=[selr, csr["own1h"]], writes=[selr])
            S.op("dve", lambda e: e.tensor_scalar(out=maskb[:], in0=sel[:], scalar1=-1.0, scalar2=-NEG, op0=ALU.add, op1=ALU.mult),
                 reads=[selr], writes=[selr])

        def ms_c(h):
            s = h % 2
            for grp in range(4):
                for q8 in range(8):
                    qt = grp * 8 + q8
                    S.op("pe", lambda e, qt=qt, q8=q8: e.transpose(out=pv7[0:16, q8 * 128:(q8 + 1) * 128], in_=maskb[:, qt * 16:(qt + 1) * 16],
                                                                   identity=cs["ident"][:]),
                         reads=[selr, csr["ident"]], writes=[psr[7]])
                S.op("act", lambda e, grp=grp: e.activation(out=brhs[s][0:16, grp * 1024:(grp + 1) * 1024], in_=pv7[0:16, 0:1024], func=AF.Copy),
                     reads=[psr[7]], writes=[brr[s]])
        load_head(0, 0)
        ms_a(0)
        ms_b(0)
        ms_c(0)
        pi = 0
        for h in range(8):
            s = h % 2
            if h + 1 < 8:
                load_head(h + 1, (h + 1) % 2)
            for Q in range(8):
                nkt = 4 * Q + 4
                ob, db = 2 + Q % 2, 4 + Q % 2
                pend = None

                def pv_mm(j, pt, ob=ob, db=db, nkt=nkt, s=s):
                    S.op("pe", lambda e: e.matmul(out=ps[ob][:], lhsT=vv[s][:, j, :], rhs=PT[pt][:], start=(j == 0), stop=(j == nkt - 1)),
                         reads=[vvr[s], PTr[pt]], writes=[psr[ob]])
                    S.op("pe", lambda e: e.matmul(out=ps[db][:], lhsT=cs["ones"][:], rhs=PT[pt][:], start=(j == 0), stop=(j == nkt - 1)),
                         reads=[csr["ones"], PTr[pt]], writes=[psr[db]])
                for j in range(nkt):
                    scb = pi % 2
                    pt = pi % 3
                    pi += 1
                    diag = j >= 4 * Q
                    S.op("pe", lambda e, j=j, Q=Q, scb=scb, s=s: e.matmul(out=ps[scb][:], lhsT=kT[s][:, j * 128:(j + 1) * 128], rhs=qT[s][:, Q * 512:(Q + 1) * 512],
                                                                         start=True, stop=False), reads=[kTr[s], qTr[s]], writes=[psr[scb]])
                    S.op("pe", lambda e, j=j, Q=Q, scb=scb, s=s, diag=diag: e.matmul(out=ps[scb][:], lhsT=cs["bl"][:, j, :], rhs=brhs[s][:, Q * 512:(Q + 1) * 512],
                                                                                    start=False, stop=(not diag)), reads=[csr["bl"], brr[s]], writes=[psr[scb]])
                    if diag:
                        c0 = (j // 2 - 2 * Q) * 256
                        S.op("pe", lambda e, j=j, scb=scb, c0=c0: e.matmul(out=ps[scb][:, c0:c0 + 256], lhsT=cs["ident"][:], rhs=cs["tri"][:, j % 2, :],
                                                                          start=False, stop=True), reads=[csr["ident"], csr["tri"]], writes=[psr[scb]])
                    S.op("act", lambda e, scb=scb, pt=pt: e.activation(out=PT[pt][:], in_=ps[scb][:], func=AF.Exp), reads=[psr[scb]], writes=[PTr[pt]])
                    if pend is not None:
                        pv_mm(*pend)
                    pend = (j, pt)
                pv_mm(*pend)
                S.op("dve", lambda e, db=db: e.reciprocal(out=rden[:], in_=ps[db][:]), reads=[psr[db]], writes=[rdenr])
                S.op("dve", lambda e, ob=ob, Q=Q: e.tensor_tensor(out=yo[Q % 2][:], in0=ps[ob][:], in1=rden[:], op=ALU.mult),
                     reads=[psr[ob], rdenr], writes=[yor[Q % 2]])
                S.dma("sp", G.scr["YTD"][8 + h, :, Q * 512:(Q + 1) * 512], yo[Q % 2][:], yor[Q % 2], reads=[yor[Q % 2]], writes=[G.dres["YTD"]])
                if h + 1 < 8:
                    if Q == 1:
                        ms_a(h + 1)
                    elif Q == 3:
                        ms_b(h + 1)
                    elif Q == 5:
                        ms_c(h + 1)
    run_phase(G, body)
```

```python
class Res:
    __slots__ = ("name", "w", "r", "dsem", "dcount", "last_dma")

    def __init__(self, name):
        self.name = name
        self.w = {}
        self.r = {}
        self.dsem = None
        self.dcount = 0
        self.last_dma = None


class Sched:
    ENG = ("pe", "dve", "act", "pool", "sp")

    def __init__(self, nc, sems):
        self.nc = nc
        self.sems = sems
        self.ops = {e: [] for e in self.ENG}
        self.esem = {e: self.sems.pop() for e in self.ENG}
        self.cnt = {e: 0 for e in self.ENG}
        self.waited = {e: {} for e in self.ENG}
        self.nwaits = 0

    def res(self, name, dma=False):
        r = Res(name)
        if dma:
            if not hasattr(self, "dpool"):
                self.dpool = []
                self.live = []
            if self.dpool:
                r.dsem, r.dcount = self.dpool.pop()
            else:
                r.dsem, r.dcount = self.sems.pop(), 0
            self.live.append(r)
        return r

    def release_all(self):
        for r in getattr(self, "live", []):
            self.dpool.append((r.dsem, r.dcount))
        self.live = []

    def barrier(self):
        evs = {}
        for e in self.ENG:
            if self.cnt[e] > 0:
                self._merge(evs, (self.esem[e], self.cnt[e]))
        for r in getattr(self, "live", []):
            if r.last_dma is not None:
                self._merge(evs, r.last_dma)
        for e in self.ENG:
            wl = self._filter(e, dict(evs))
            self.ops[e].append((wl, None, None))

    @staticmethod
    def _merge(d, ev):
        k = id(ev[0])
        if k not in d or d[k][1] < ev[1]:
            d[k] = ev

    def _collect(self, reads, writes):
        waits = {}
        for r in reads:
            for ev in r.w.values():
                self._merge(waits, ev)
        for w in writes:
            for ev in w.w.values():
                self._merge(waits, ev)
            for ev in w.r.values():
                self._merge(waits, ev)
        return waits

    def _filter(self, eng, waits, skip_self=False):
        out = []
        wd = self.waited[eng]
        for k, (sem, val) in waits.items():
            if skip_self and sem is self.esem[eng]:
                continue
            if wd.get(k, 0) >= val:
                continue
            wd[k] = val
            out.append((sem, val))
        self.nwaits += len(out)
        return out

    def op(self, eng, fn, reads=(), writes=()):
        waits = self._collect(reads, writes)
        wl = self._filter(eng, waits, skip_self=(eng == "pe"))
        self.cnt[eng] += 1
        ev = (self.esem[eng], self.cnt[eng])
        for r in reads:
            self._merge(r.r, ev)
        for w in writes:
            w.w = {id(ev[0]): ev}
            w.r = {}
        self.ops[eng].append((wl, fn, (self.esem[eng], 1)))

    def dma(self, eng, out, in_, slot, reads=(), writes=(), **kw):
        assert slot.dsem is not None, slot.name
        waits = self._collect(reads, writes)
        if slot.last_dma is not None:
            self._merge(waits, slot.last_dma)
        wl = self._filter(eng, waits)
        slot.dcount += 16
        ev = (slot.dsem, slot.dcount)
        slot.last_dma = ev
        for r in reads:
            self._merge(r.r, ev)
        for w in writes:
            if w is slot:
                w.w = {id(ev[0]): ev}
            else:
                self._merge(w.w, ev)
            w.r = {}

        def fn(e, out=out, in_=in_, kw=kw):
            return e.dma_start(out=out, in_=in_, **kw)

        self.ops[eng].append((wl, fn, (slot.dsem, 16)))

    def custom(self, eng, fn, sem_res, reads=(), writes=(), inc=16):
        waits = self._collect(reads, writes)
        if sem_res.last_dma is not None:
            self._merge(waits, sem_res.last_dma)
        wl = self._filter(eng, waits)
        sem_res.dcount += inc
        ev = (sem_res.dsem, sem_res.dcount)
        sem_res.last_dma = ev
        for r in reads:
            self._merge(r.r, ev)
        for w in writes:
            w.w = {id(ev[0]): ev}
            w.r = {}
        self.ops[eng].append((wl, fn, (sem_res.dsem, inc)))

    def final_wait(self, eng, resources):
        waits = self._collect(resources, resources)
        wl = self._filter(eng, waits)
        self.ops[eng].append((wl, None, None))

    def emit(self, block):
        names = {"pe": "tensor", "dve": "vector", "act": "scalar", "pool": "gpsimd", "sp": "sync"}
        for e in self.ENG:
            ops = self.ops[e]
            self.ops[e] = []

            def body(engine, ops=ops):
                for wl, fn, inc in ops:
                    for sem, val in wl:
                        engine.wait_ge(sem, val)
                    if fn is not None:
                        ins = fn(engine)
                        ins.then_inc(inc[0], inc[1])

            getattr(block, names[e])(body)
import numpy as np
import ml_dtypes
from contextlib import ExitStack
import concourse.bass as bass
import concourse.mybir as mybir
from concourse.bass_utils import run_bass_kernel_spmd

F32 = mybir.dt.float32
BF16 = mybir.dt.bfloat16
AF = mybir.ActivationFunctionType
ALU = mybir.AluOpType
AX = mybir.AxisListType

NCORES = 4
T = 4096
D = 2048
KC = 16
DFF = 5632
FT = 44
DEPTH = 2
EPS = 1e-6
GAMMA = [1.0 - 2.0 ** (-5.0 - h) for h in range(4)]
SLOPES = [2.0 ** (-(h + 1)) for h in range(8)]
NEG = -30000.0


def host_consts():
    c = {}
    bf = ml_dtypes.bfloat16
    c["ident"] = np.eye(128, dtype=np.float32).astype(bf)
    c["ones"] = np.ones((128, 128), np.float32).astype(bf)
    c["identf"] = np.eye(128, dtype=np.float32)
    p = np.arange(128)
    kd = np.zeros((128, 2, 4), np.float32)
    qd = np.zeros((128, 2, 4), np.float32)
    dec = np.zeros((128, 4, 2, 256), np.float32)
    cc = np.arange(256)
    for h in range(4):
        lg = np.log(np.float32(GAMMA[h])).astype(np.float32)
        for par in range(2):
            pos = par * 128 + p
            kd[:, par, h] = np.exp(lg * (255.0 - pos)) / 16.0
            qd[:, par, h] = np.exp(lg * (pos + 1.0))
            diff = cc[None, :] - pos[:, None]
            dec[:, h, par, :] = np.where(diff >= 0, np.exp(lg * np.maximum(diff, 0)), 0.0) / 16.0
    c["kd"] = kd
    c["qd"] = qd
    c["dec"] = dec
    qt = np.arange(32)
    slot = np.arange(16)
    valid = (slot[None, :] < (qt // 2)[:, None]).astype(np.float32)
    own = (slot[None, :] == (qt // 2)[:, None]).astype(np.float32)
    c["negb"] = np.broadcast_to(np.where(valid > 0, 0.0, -1e30).astype(np.float32)[None], (128, 32, 16)).copy()
    c["validf"] = np.broadcast_to(valid[None], (128, 32, 16)).copy()
    c["own1h"] = np.broadcast_to(own[None], (128, 32, 16)).copy()
    bl = np.zeros((128, 32, 128), np.float32)
    for j in range(32):
        bl[j // 2, j, :] = 1.0
        bl[32, j, :] = 1.0
        bl[33, j, :] = 1.0
        bl[34, j, :] = np.arange(128)
        bl[35, j, :] = j
    c["bl"] = bl.astype(bf)
    brc = np.zeros((8, 4, T), np.float32)
    t = np.arange(T)
    for h in range(8):
        s = SLOPES[h]
        brc[h, 0] = -s * (t % 256)
        brc[h, 1] = -s * 256.0 * (t // 256)
        brc[h, 2] = s
        brc[h, 3] = s * 128.0
    c["brc"] = brc.astype(bf)
    tri = np.zeros((128, 2, 256), np.float32)
    for a in range(2):
        tri[:, a, :] = np.where((p[:, None] + 128 * a) > cc[None, :], NEG, 0.0)
    c["tri"] = tri.astype(bf)
    return c


CONST_SHAPES = {
    "identf": ([128, 128], F32), "ident": ([128, 128], BF16), "ones": ([128, 128], BF16), "kd": ([128, 2, 4], F32), "qd": ([128, 2, 4], F32),
    "dec": ([128, 4, 2, 256], F32), "negb": ([128, 32, 16], F32), "validf": ([128, 32, 16], F32),
    "own1h": ([128, 32, 16], F32), "bl": ([128, 32, 128], BF16), "brc": ([8, 4, T], BF16), "tri": ([128, 2, 256], BF16),
}

IN_SHAPES = {
    "x": [T, D], "ln1_g": [DEPTH, D], "w_in": [DEPTH, D, 7168], "ret_norm_g": [DEPTH, 1024], "q_norm_g": [DEPTH, 128],
    "k_norm_g": [DEPTH, 128], "w_out": [DEPTH, D, D], "ln2_g": [DEPTH, D], "w_gate": [DEPTH, D, DFF],
    "w_up": [DEPTH, D, DFF], "conv_w": [DEPTH, 3, DFF], "conv_b": [DEPTH, DFF], "w_down": [DEPTH, DFF, D],
}

SCRATCH = {
    "RQT": ([8, 128, T], BF16), "RKT": ([8, 128, T], BF16), "RK": ([T, 1024], BF16), "RV": ([T, 1024], BF16),
    "SG": ([T, 1024], F32), "MQT": ([8, 128, T], BF16), "MKT": ([8, 128, T], BF16), "MV": ([T, 1024], BF16),
    "YTD": ([16, 128, T], BF16), "XR": ([T, D], F32), "XO": ([T, D], F32), "GT": ([FT, 128, T], BF16),
}


class Ctx:
    pass


DBG = {}


def build(n_layers=DEPTH, stop_after=None, debug=False):
    nc = bass.Bass("TRN2", target_bir_lowering=False)
    G = Ctx()
    G.nc = nc
    G.inp = {k: nc.dram_tensor(k, v, F32, kind="ExternalInput").ap() for k, v in IN_SHAPES.items()}
    G.cst = {k: nc.dram_tensor("c_" + k, v[0], v[1], kind="ExternalInput").ap() for k, v in CONST_SHAPES.items()}
    G.out = nc.dram_tensor("out", [T, D], F32, kind="ExternalOutput").ap()
    G.scr = {k: nc.dram_tensor("s_" + k, v[0], v[1], kind=("ExternalOutput" if debug else "Internal")).ap()
             for k, v in SCRATCH.items()}
    with ExitStack() as st:
        sems = [st.enter_context(nc.semaphore(f"sem{i}")) for i in range(100)]
        S = Sched(nc, sems)
        G.S = S
        G.ps = [st.enter_context(nc.psum_tensor(f"ps{i}", [128, 512], F32)) for i in range(8)]
        G.dres = {k: S.res("d_" + k) for k in list(SCRATCH) + ["x", "out", "w"]}
        G.halo = st.enter_context(nc.sbuf_tensor("halo", [128, FT, 2], F32))
        G.halor = S.res("halo")
        phases = []
        for l in range(n_layers):
            xsrc = (G.inp["x"], G.dres["x"]) if l == 0 else (G.scr["XO"], G.dres["XO"])
            last = (l == n_layers - 1)
            xdst = (G.out, G.dres["out"]) if last else (G.scr["XO"], G.dres["XO"])
            for hf in range(2):
                phases.append(("AB", lambda l=l, hf=hf, xsrc=xsrc: phase_AB(G, l, hf, xsrc)))
            phases.append(("C", lambda l=l: phase_C(G, l)))
            phases.append(("D", lambda l=l: phase_D(G, l)))
            for hf in range(2):
                phases.append(("E", lambda l=l, hf=hf, xsrc=xsrc: phase_E(G, l, hf, xsrc)))
            for hf in range(2):
                phases.append(("FG", lambda l=l, hf=hf: phase_FG(G, l, hf)))
            phases.append(("H", lambda l=l, xdst=xdst: phase_H(G, l, 0, xdst)))
        for name, fn in phases:
            fn()
            if stop_after is not None and name == stop_after:
                break
        with ExitStack() as ph:
            S.barrier()
            blk = ph.enter_context(nc.Block())
            S.emit(blk)
    return nc


def run_phase(G, fn):
    nc, S = G.nc, G.S
    with ExitStack() as ph:
        S.barrier()
        S.release_all()
        fn(ph)
        blk = ph.enter_context(nc.Block())
        S.emit(blk)


_UID = [0]


def sb(ph, G, name, shape, dt):
    _UID[0] += 1
    return ph.enter_context(G.nc.sbuf_tensor(f"t{_UID[0]}_{name}", shape, dt))


def weight_steps(G, wb, wbr, stg, stgr, kctr, wsrc, nkc, wres):
    S = G.S
    ncols = wsrc.shape[1]
    wv = wsrc.rearrange("(kc p) n -> p kc n", p=128)
    gsz = stg[0].shape[1]
    steps = []
    g0 = 0
    while g0 < nkc:
        g1 = min(nkc, g0 + gsz)

        def step(g0=g0, g1=g1):
            sl = kctr[0] % len(stg)
            kctr[0] += 1
            S.dma("sp", stg[sl][:, 0:g1 - g0, 0:ncols], wv[:, g0:g1, :], stgr[sl], reads=[wres], writes=[stgr[sl]])
            S.op("pool", lambda e, sl=sl: e.tensor_copy(out=wb[:, g0:g1, 0:ncols], in_=stg[sl][:, 0:g1 - g0, 0:ncols]),
                 reads=[stgr[sl]], writes=[wbr])
        steps.append(step)
        g0 = g1
    return steps


def weight_steps2(G, wb, wbr, stg, stgr, kctr, wsrc, nkc, wres, eng="act"):
    S = G.S
    ncols = wsrc.shape[1]
    wv = wsrc.rearrange("(kc p) n -> p kc n", p=128)
    gsz = stg[0].shape[1]
    steps = []
    g0 = 0
    while g0 < nkc:
        g1 = min(nkc, g0 + gsz)
        box = {}

        def dma_fn(g0=g0, g1=g1, box=box):
            sl = kctr[0] % len(stg)
            kctr[0] += 1
            box["sl"] = sl
            S.dma("sp", stg[sl][:, 0:g1 - g0, 0:ncols], wv[:, g0:g1, :], stgr[sl], reads=[wres], writes=[stgr[sl]])

        def cast_fn(g0=g0, g1=g1, box=box):
            sl = box["sl"]
            if eng == "act":
                S.op("act", lambda e: e.activation(out=wb[:, g0:g1, 0:ncols], in_=stg[sl][:, 0:g1 - g0, 0:ncols], func=AF.Copy),
                     reads=[stgr[sl]], writes=[wbr])
            else:
                S.op(eng, lambda e: e.tensor_copy(out=wb[:, g0:g1, 0:ncols], in_=stg[sl][:, 0:g1 - g0, 0:ncols]),
                     reads=[stgr[sl]], writes=[wbr])
        steps.append((dma_fn, cast_fn))
        g0 = g1
    return steps


def load_weight_block(G, wb, wbr, stg, stgr, kctr, wsrc, nkc, wres):
    for st_ in weight_steps(G, wb, wbr, stg, stgr, kctr, wsrc, nkc, wres):
        st_()


def phase_AB(G, l, hf, xsrc):
    def body(ph):
        nc, S = G.nc, G.S
        xap, xres = xsrc
        HT = sb(ph, G, "HT", [128, KC, 2048], BF16)
        HTr = S.res("HT")
        WB = [sb(ph, G, f"WB{i}", [128, KC, 512], BF16) for i in range(2)]
        WBr = [S.res(f"WB{i}") for i in range(2)]
        STG = [sb(ph, G, f"STG{i}", [128, 4, 512], F32) for i in range(4)]
        STGr = [S.res(f"STG{i}", dma=True) for i in range(4)]
        kctr = [0]
        xt = [sb(ph, G, f"xt{i}", [128, D], F32) for i in range(4)]
        xtr = [S.res(f"xt{i}", dma=True) for i in range(4)]
        hb = [sb(ph, G, f"hb{i}", [128, D], BF16) for i in range(2)]
        hbr = [S.res(f"hb{i}") for i in range(2)]
        g1b = sb(ph, G, "g1b", [128, D], F32)
        g1r = S.res("g1b", dma=True)
        junk = sb(ph, G, "junk", [128, D], BF16)
        junkr = S.res("junk")
        sm = sb(ph, G, "sm", [128, 64], F32)
        smr = [S.res(f"sm{i}") for i in range(8)]
        cs = {}
        csr = {}
        for k in ("ident", "kd"):
            cs[k] = sb(ph, G, "c_" + k, CONST_SHAPES[k][0], CONST_SHAPES[k][1])
            csr[k] = S.res("c_" + k, dma=True)
            S.dma("sp", cs[k][:], G.cst[k], csr[k], writes=[csr[k]])
        gq = sb(ph, G, "gq", [128, 2, 128], F32)
        gqr = S.res("gq", dma=True)
        S.dma("sp", gq[:, 0, :], G.inp["q_norm_g"][l].partition_broadcast(128), gqr, writes=[gqr])
        S.dma("sp", gq[:, 1, :], G.inp["k_norm_g"][l].partition_broadcast(128), gqr, writes=[gqr])
        S.op("dve", lambda e: e.tensor_scalar(out=gq[:, 0, :], in0=gq[:, 0, :], scalar1=float(128 ** -0.5), scalar2=None, op0=ALU.mult),
             reads=[gqr], writes=[gqr])
        S.dma("sp", g1b[:], G.inp["ln1_g"][l].partition_broadcast(128), g1r, writes=[g1r])
        ps = G.ps
        psr = [S.res(f"ps{i}") for i in range(8)]
        win = G.inp["w_in"][l]
        wres = G.dres["w"]
        load_weight_block(G, WB[0], WBr[0], STG, STGr, kctr, win[:, 0:512], KC, wres)
        def n_stats(tt):
            gt = hf * 16 + tt
            b = tt % 2
            xb = tt % 4
            if tt == 0:
                for t_ in range(2):
                    S.dma("sp", xt[t_][:], xap[(gt + t_) * 128:(gt + t_ + 1) * 128, :], xtr[t_], reads=[xres], writes=[xtr[t_]])
            if tt + 2 < 16:
                S.dma("sp", xt[(tt + 2) % 4][:], xap[(gt + 2) * 128:(gt + 3) * 128, :], xtr[(tt + 2) % 4], reads=[xres], writes=[xtr[(tt + 2) % 4]])
            c0 = (tt % 4) * 3
            ss, lnv, rstd = sm[:, c0:c0 + 1], sm[:, c0 + 1:c0 + 2], sm[:, c0 + 2:c0 + 3]
            r_s = smr[tt % 4]
            S.op("act", lambda e, xb=xb, ss=ss: e.activation(out=junk[:], in_=xt[xb][:], func=AF.Square, accum_out=ss),
                 reads=[xtr[xb]], writes=[junkr, r_s])
            S.op("act", lambda e, ss=ss, lnv=lnv: e.activation(out=lnv, in_=ss, func=AF.Ln, scale=1.0 / D, bias=EPS),
                 reads=[r_s], writes=[r_s])
            S.op("act", lambda e, lnv=lnv, rstd=rstd: e.activation(out=rstd, in_=lnv, func=AF.Exp, scale=-0.5),
                 reads=[r_s], writes=[r_s])

        def n_tail(tt):
            gt = hf * 16 + tt
            b = tt % 2
            xb = tt % 4
            c0 = (tt % 4) * 3
            ss, lnv, rstd = sm[:, c0:c0 + 1], sm[:, c0 + 1:c0 + 2], sm[:, c0 + 2:c0 + 3]
            r_s = smr[tt % 4]
            S.op("dve", lambda e, b=b, xb=xb, rstd=rstd: e.scalar_tensor_tensor(out=hb[b][:], in0=xt[xb][:], scalar=rstd, in1=g1b[:],
                                                                        op0=ALU.mult, op1=ALU.mult),
                 reads=[xtr[xb], r_s, g1r], writes=[hbr[b]])
            pa, pb = 3 + 2 * (tt % 2), 4 + 2 * (tt % 2)
            for kc in range(16):
                bank = pa if kc < 8 else pb
                pv = ps[bank][:].bitcast(BF16)
                S.op("pe", lambda e, b=b, kc=kc, pv=pv: e.transpose(out=pv[:, (kc % 8) * 128:(kc % 8 + 1) * 128],
                                                                     in_=hb[b][:, kc * 128:(kc + 1) * 128], identity=cs["ident"][:]),
                     reads=[hbr[b], csr["ident"]], writes=[psr[bank]])
            S.op("act", lambda e, tt=tt, pa=pa: e.activation(out=HT[:, 0:8, tt * 128:(tt + 1) * 128],
                                                             in_=ps[pa][:].bitcast(BF16).rearrange("p (k t) -> p k t", k=8), func=AF.Copy),
                 reads=[psr[pa]], writes=[HTr])
            S.op("dve", lambda e, tt=tt, pb=pb: e.tensor_copy(out=HT[:, 8:16, tt * 128:(tt + 1) * 128],
                                                              in_=ps[pb][:].bitcast(BF16).rearrange("p (k t) -> p k t", k=8)),
                 reads=[psr[pb]], writes=[HTr])
        n_stats(0)
        for tt in range(16):
            if tt + 1 < 16:
                n_stats(tt + 1)
            n_tail(tt)
        if DBG.get('stopA'):
            return
        NCB = DBG.get('ncb', 14)
        sb16 = [sb(ph, G, f"sb16_{i}", [128, 512], BF16) for i in range(3)]
        sb16r = [S.res(f"sb16_{i}") for i in range(3)]
        c32 = [sb(ph, G, f"c32_{i}", [128, 512], F32) for i in range(2)]
        c32r = [S.res(f"c32_{i}") for i in range(2)]
        tmo = [sb(ph, G, f"tmo{i}", [128, 512], F32) for i in range(3)]
        tmor = [S.res(f"tmo{i}", dma=True) for i in range(3)]
        fst = [sb(ph, G, f"fst{i}", [128, 4, 512], BF16) for i in range(2)]
        fstr = [S.res(f"fst{i}", dma=True) for i in range(2)]
        it = 0
        tmc = 0
        wsteps = []
        pend_cast = []
        tailq = []
        for cb in range(14):
            if cb + 1 < 14:
                wsteps = weight_steps2(G, WB[(cb + 1) % 2], WBr[(cb + 1) % 2], STG, STGr, kctr, win[:, (cb + 1) * 512:(cb + 2) * 512], KC, wres)
            w = WB[cb % 2]
            wr = WBr[cb % 2]
            for tt in range(16):
                gt = hf * 16 + tt
                par = gt % 2
                bank = it % 3
                it += 1
                while pend_cast:
                    pend_cast.pop(0)()
                if wsteps:
                    d_, c_ = wsteps.pop(0)
                    d_()
                    pend_cast.append(c_)
                for kc in range(16):
                    S.op("pe", lambda e, kc=kc, tt=tt, bank=bank, w=w: e.matmul(out=ps[bank][:], lhsT=HT[:, kc, tt * 128:(tt + 1) * 128],
                                                                                rhs=w[:, kc, :], start=(kc == 0), stop=(kc == 15)),
                         reads=[HTr, wr], writes=[psr[bank]])
                while len(tailq) > 1:
                    tailq.pop(0)()
                P = ps[bank]
                Pr = psr[bank]
                fm = cb in (0, 1, 2, 3, 8, 9, 10, 11)
                i2 = it % 2
                i3 = it % 3
                if cb in (0, 1, 2, 3):
                    S.op("act", lambda e, i3=i3, P=P: e.activation(out=sb16[i3][:], in_=P[:], func=AF.Copy),
                         reads=[Pr], writes=[sb16r[i3]])
                if cb in (2, 3):
                    ts = tmc % 3
                    tmc += 1
                    tv = tmo[ts][:].bitcast(BF16)[:, 0:512]
                    for hh in range(2):
                        h = (cb - 2) * 2 + hh
                        S.op("dve", lambda e, hh=hh, h=h, P=P, tv=tv, par=par: e.tensor_scalar(
                            out=tv[:, hh * 256:(hh + 1) * 256], in0=P[:, hh * 256:(hh + 1) * 256],
                            scalar1=cs["kd"][:, par, h:h + 1], scalar2=None, op0=ALU.mult),
                            reads=[Pr, csr["kd"], sb16r[i3]], writes=[tmor[ts]])
                    S.dma("sp", G.scr["RK"][gt * 128:(gt + 1) * 128, (cb - 2) * 512:(cb - 1) * 512], tv, tmor[ts],
                          reads=[tmor[ts]], writes=[G.dres["RK"]])
                if cb in (4, 5, 12, 13):
                    ts = tmc % 3
                    tmc += 1
                    tv = tmo[ts][:].bitcast(BF16)[:, 0:512]
                    if it % 2 == 0:
                        S.op("act", lambda e, P=P, tv=tv: e.activation(out=tv, in_=P[:], func=AF.Copy), reads=[Pr], writes=[tmor[ts]])
                    else:
                        S.op("dve", lambda e, P=P, tv=tv: e.tensor_copy(out=tv, in_=P[:]), reads=[Pr], writes=[tmor[ts]])
                    dst = G.scr["RV"] if cb < 8 else G.scr["MV"]
                    dr = G.dres["RV"] if cb < 8 else G.dres["MV"]
                    c0 = (cb - 4) * 512 if cb < 8 else (cb - 12) * 512
                    S.dma("sp", dst[gt * 128:(gt + 1) * 128, c0:c0 + 512], tv, tmor[ts], reads=[tmor[ts]], writes=[dr])
                if cb in (6, 7):
                    ts = tmc % 3
                    tmc += 1
                    S.op("act", lambda e, P=P, ts=ts: e.activation(out=tmo[ts][:], in_=P[:], func=AF.Silu), reads=[Pr], writes=[tmor[ts]])
                    S.dma("sp", G.scr["SG"][gt * 128:(gt + 1) * 128, (cb - 6) * 512:(cb - 5) * 512], tmo[ts][:], tmor[ts],
                          reads=[tmor[ts]], writes=[G.dres["SG"]])
                if cb in (8, 9, 10, 11):
                    gi = 0 if cb < 10 else 1
                    c0 = 12 + (it % 2) * 8
                    ss4, ln4, r4 = sm[:, c0:c0 + 4], sm[:, c0 + 4:c0 + 8], sm[:, c0 + 4:c0 + 8]
                    r_s = smr[4 + it % 2]
                    S.op("act", lambda e, i2=i2, P=P: e.activation(out=c32[i2][:], in_=P[:], func=AF.Copy), reads=[Pr], writes=[c32r[i2]])
                    for hh in range(4):
                        S.op("act", lambda e, i2=i2, hh=hh, ss4=ss4: e.activation(out=junk[:, hh * 128:(hh + 1) * 128], in_=c32[i2][:, hh * 128:(hh + 1) * 128],
                                                                                 func=AF.Square, accum_out=ss4[:, hh:hh + 1]),
                             reads=[c32r[i2]], writes=[junkr, r_s])
                    S.op("act", lambda e, ss4=ss4, ln4=ln4: e.activation(out=ln4, in_=ss4, func=AF.Ln, scale=1.0 / 128, bias=EPS),
                         reads=[r_s], writes=[r_s])
                    S.op("act", lambda e, ln4=ln4, r4=r4: e.activation(out=r4, in_=ln4, func=AF.Exp, scale=-0.5), reads=[r_s], writes=[r_s])
                    for hh in range(4):
                        S.op("dve", lambda e, i2=i2, i3=i3, hh=hh, r4=r4, gi=gi: e.scalar_tensor_tensor(
                            out=sb16[i3][:, hh * 128:(hh + 1) * 128], in0=c32[i2][:, hh * 128:(hh + 1) * 128], scalar=r4[:, hh:hh + 1],
                            in1=gq[:, gi, :], op0=ALU.mult, op1=ALU.mult),
                            reads=[c32r[i2], r_s, gqr], writes=[sb16r[i3]])
                if fm:
                    def fm_tail(cb=cb, tt=tt, i3=i3, it=it):
                        tb = 3 + (it % 4)
                        pv = ps[tb][:].bitcast(BF16)
                        for j in range(4):
                            S.op("pe", lambda e, j=j: e.transpose(out=pv[:, j * 128:(j + 1) * 128], in_=sb16[i3][:, j * 128:(j + 1) * 128],
                                                                  identity=cs["ident"][:]),
                                 reads=[sb16r[i3], csr["ident"]], writes=[psr[tb]])
                        fs = (tt // 4) % 2
                        src = pv[:, 0:512].rearrange("p (j t) -> p j t", j=4)
                        dstv = fst[fs][:, :, (tt % 4) * 128:(tt % 4 + 1) * 128]
                        if it % 2 == 0:
                            S.op("act", lambda e: e.activation(out=dstv, in_=src, func=AF.Copy), reads=[psr[tb]], writes=[fstr[fs]])
                        else:
                            S.op("dve", lambda e: e.tensor_copy(out=dstv, in_=src), reads=[psr[tb]], writes=[fstr[fs]])
                        if tt % 4 == 3:
                            nm, cbase = {0: ("RQT", 0), 1: ("RQT", 4), 2: ("RKT", 0), 3: ("RKT", 4), 8: ("MQT", 0), 9: ("MQT", 4),
                                         10: ("MKT", 0), 11: ("MKT", 4)}[cb]
                            tok0 = hf * 2048 + (tt // 4) * 512
                            S.dma("sp", G.scr[nm][cbase:cbase + 4, :, tok0:tok0 + 512].rearrange("c p t -> p c t"), fst[fs][:], fstr[fs],
                                  reads=[fstr[fs]], writes=[G.dres[nm]])
                    tailq.append(fm_tail)
        while tailq:
            tailq.pop(0)()
    run_phase(G, body)


def make_in_maps(inputs):
    cst = host_consts()
    maps = []
    for c in range(NCORES):
        m = {}
        for k, shp in IN_SHAPES.items():
            a = np.asarray(inputs[k])
            if k == "x":
                a = a[c]
            m[k] = np.ascontiguousarray(a.reshape(shp), dtype=np.float32)
        for k, v in cst.items():
            m["c_" + k] = v
        maps.append(m)
    return maps


_NC_CACHE = {}


def kernel(**inputs):
    if "nc" not in _NC_CACHE:
        _NC_CACHE["nc"] = build()
    nc = _NC_CACHE["nc"]
    res = run_bass_kernel_spmd(nc, make_in_maps(inputs), core_ids=list(range(NCORES)))
    return np.stack([np.asarray(res.results[c]["out"]).reshape(T, D) for c in range(NCORES)], axis=0).astype(np.float32)


def norm_to_HT(G, ph, S, HT, HTr, xap, xres, gain_ap, hf, ident, identr, psr):
    ps = G.ps
    xt = [sb(ph, G, f"xt{i}", [128, D], F32) for i in range(4)]
    xtr = [S.res(f"xt{i}", dma=True) for i in range(4)]
    hb = [sb(ph, G, f"hb{i}", [128, D], BF16) for i in range(2)]
    hbr = [S.res(f"hb{i}") for i in range(2)]
    g1b = sb(ph, G, "g1b", [128, D], F32)
    g1r = S.res("g1b", dma=True)
    junk = sb(ph, G, "junkn", [128, D], BF16)
    junkr = S.res("junkn")
    sm = sb(ph, G, "smn", [128, 16], F32)
    smr = [S.res(f"smn{i}") for i in range(4)]
    S.dma("sp", g1b[:], gain_ap.partition_broadcast(128), g1r, writes=[g1r])
    def n_stats(tt):
        gt = hf * 16 + tt
        b = tt % 2
        xb = tt % 4
        if tt == 0:
            for t_ in range(2):
                S.dma("sp", xt[t_][:], xap[(gt + t_) * 128:(gt + t_ + 1) * 128, :], xtr[t_], reads=[xres], writes=[xtr[t_]])
        if tt + 2 < 16:
            S.dma("sp", xt[(tt + 2) % 4][:], xap[(gt + 2) * 128:(gt + 3) * 128, :], xtr[(tt + 2) % 4], reads=[xres], writes=[xtr[(tt + 2) % 4]])
        c0 = (tt % 4) * 3
        ss, lnv, rstd = sm[:, c0:c0 + 1], sm[:, c0 + 1:c0 + 2], sm[:, c0 + 2:c0 + 3]
        r_s = smr[tt % 4]
        S.op("act", lambda e, xb=xb, ss=ss: e.activation(out=junk[:], in_=xt[xb][:], func=AF.Square, accum_out=ss),
             reads=[xtr[xb]], writes=[junkr, r_s])
        S.op("act", lambda e, ss=ss, lnv=lnv: e.activation(out=lnv, in_=ss, func=AF.Ln, scale=1.0 / D, bias=EPS), reads=[r_s], writes=[r_s])
        S.op("act", lambda e, lnv=lnv, rstd=rstd: e.activation(out=rstd, in_=lnv, func=AF.Exp, scale=-0.5), reads=[r_s], writes=[r_s])

    def n_tail(tt):
        gt = hf * 16 + tt
        b = tt % 2
        xb = tt % 4
        c0 = (tt % 4) * 3
        ss, lnv, rstd = sm[:, c0:c0 + 1], sm[:, c0 + 1:c0 + 2], sm[:, c0 + 2:c0 + 3]
        r_s = smr[tt % 4]
        S.op("dve", lambda e, b=b, xb=xb, rstd=rstd: e.scalar_tensor_tensor(out=hb[b][:], in0=xt[xb][:], scalar=rstd, in1=g1b[:],
                                                                    op0=ALU.mult, op1=ALU.mult),
             reads=[xtr[xb], r_s, g1r], writes=[hbr[b]])
        pa, pb = 3 + 2 * (tt % 2), 4 + 2 * (tt % 2)
        for kc in range(16):
            bank = pa if kc < 8 else pb
            pv = ps[bank][:].bitcast(BF16)
            S.op("pe", lambda e, b=b, kc=kc, pv=pv: e.transpose(out=pv[:, (kc % 8) * 128:(kc % 8 + 1) * 128],
                                                                 in_=hb[b][:, kc * 128:(kc + 1) * 128], identity=ident[:]),
                 reads=[hbr[b], identr], writes=[psr[bank]])
        S.op("act", lambda e, tt=tt, pa=pa: e.activation(out=HT[:, 0:8, tt * 128:(tt + 1) * 128],
                                                         in_=ps[pa][:].bitcast(BF16).rearrange("p (k t) -> p k t", k=8), func=AF.Copy),
             reads=[psr[pa]], writes=[HTr])
        S.op("dve", lambda e, tt=tt, pb=pb: e.tensor_copy(out=HT[:, 8:16, tt * 128:(tt + 1) * 128],
                                                          in_=ps[pb][:].bitcast(BF16).rearrange("p (k t) -> p k t", k=8)),
             reads=[psr[pb]], writes=[HTr])
    n_stats(0)
    for tt in range(16):
        if tt + 1 < 16:
            n_stats(tt + 1)
        n_tail(tt)


def phase_E(G, l, hf, xsrc):
    def body(ph):
        S = G.S
        ps = G.ps
        xap, xres = xsrc
        HT = sb(ph, G, "YT", [128, KC, 2048], BF16)
        HTq = [S.res(f"YTq{i}", dma=True) for i in range(4)]
        WB = [sb(ph, G, f"WB{i}", [128, KC, 512], BF16) for i in range(2)]
        WBr = [S.res(f"WB{i}") for i in range(2)]
        STG = [sb(ph, G, f"STG{i}", [128, 4, 512], F32) for i in range(4)]
        STGr = [S.res(f"STG{i}", dma=True) for i in range(4)]
        kctr = [0]
        xa = [sb(ph, G, f"xa{i}", [128, 512], F32) for i in range(4)]
        xar = [S.res(f"xa{i}", dma=True) for i in range(4)]
        psr = [S.res(f"ps{i}") for i in range(8)]
        w_out = G.inp["w_out"][l]
        load_weight_block(G, WB[0], WBr[0], STG, STGr, kctr, w_out[:, 0:512], KC, G.dres["w"])
        for qi in range(4):
            S.dma("sp", HT[:, :, qi * 512:(qi + 1) * 512], G.scr["YTD"][:, :, hf * 2048 + qi * 512:hf * 2048 + (qi + 1) * 512].rearrange("c p t -> p c t"),
                  HTq[qi], reads=[G.dres["YTD"]], writes=[HTq[qi]])
        iters = [(cb, tt) for cb in range(4) for tt in range(16)]

        def ldx(i):
            cb, tt = iters[i]
            gt = hf * 16 + tt
            S.dma("sp", xa[i % 4][:], xap[gt * 128:(gt + 1) * 128, cb * 512:(cb + 1) * 512], xar[i % 4], reads=[xres], writes=[xar[i % 4]])
        ldx(0)
        ldx(1)
        for it, (cb, tt) in enumerate(iters):
            if tt == 0:
                wsteps = weight_steps(G, WB[(cb + 1) % 2], WBr[(cb + 1) % 2], STG, STGr, kctr, w_out[:, (cb + 1) * 512:(cb + 2) * 512], KC, G.dres["w"]) if cb + 1 < 4 else []
            if wsteps:
                wsteps.pop(0)()
            if it + 2 < len(iters):
                ldx(it + 2)
            w, wr = WB[cb % 2], WBr[cb % 2]
            gt = hf * 16 + tt
            bank = it % 4
            xs = it % 4
            for kc in range(16):
                S.op("pe", lambda e, kc=kc, tt=tt, bank=bank, w=w: e.matmul(out=ps[bank][:], lhsT=HT[:, kc, tt * 128:(tt + 1) * 128],
                                                                            rhs=w[:, kc, :], start=(kc == 0), stop=(kc == 15)),
                     reads=[HTq[tt // 4], wr], writes=[psr[bank]])
            S.op("dve", lambda e, xs=xs, bank=bank: e.tensor_tensor(out=xa[xs][:], in0=ps[bank][:], in1=xa[xs][:], op=ALU.add),
                 reads=[psr[bank], xar[xs]], writes=[xar[xs]])
            S.dma("sp", G.scr["XR"][gt * 128:(gt + 1) * 128, cb * 512:(cb + 1) * 512], xa[xs][:], xar[xs],
                  reads=[xar[xs]], writes=[G.dres["XR"]])
    run_phase(G, body)


def phase_FG(G, l, hf):
    def body(ph):
        S = G.S
        ps = G.ps
        HT = sb(ph, G, "HT", [128, KC, 2048], BF16)
        HTr = S.res("HT")
        psr = [S.res(f"ps{i}") for i in range(8)]
        ident = sb(ph, G, "ident", [128, 128], BF16)
        identr = S.res("ident", dma=True)
        S.dma("sp", ident[:], G.cst["ident"], identr, writes=[identr])
        with ExitStack() as sub:
            norm_to_HT(G, sub, S, HT, HTr, G.scr["XR"], G.dres["XR"], G.inp["ln2_g"][l], hf, ident, identr, psr)
            S.barrier()
        identf = sb(ph, G, "identf", [128, 128], F32)
        identfr = S.res("identf", dma=True)
        S.dma("sp", identf[:], G.cst["identf"], identfr, writes=[identfr])
        cwT = sb(ph, G, "cwT", [FT, 4, 128], F32)
        cwTr = S.res("cwT", dma=True)
        S.dma("sp", cwT[:, 0:3, :], G.inp["conv_w"][l].rearrange("j (f p) -> f j p", p=128), cwTr, writes=[cwTr])
        S.dma("sp", cwT[:, 3, :], G.inp["conv_b"][l].rearrange("(f p) -> f p", p=128), cwTr, writes=[cwTr])
        cw = sb(ph, G, "cw", [128, 4, FT], F32)
        cwr = S.res("cw")
        for j in range(4):
            S.op("pe", lambda e, j=j: e.transpose(out=ps[7][:, j * FT:(j + 1) * FT], in_=cwT[:, j, :], identity=identf[0:FT, 0:FT]),
                 reads=[cwTr, identfr], writes=[psr[7]])
        S.op("dve", lambda e: e.tensor_copy(out=cw[:].rearrange("p j f -> p (j f)"), in_=ps[7][:, 0:4 * FT]), reads=[psr[7]], writes=[cwr])
        WG = [sb(ph, G, f"WG{i}", [128, KC, 512], BF16) for i in range(2)]
        WGr = [S.res(f"WG{i}") for i in range(2)]
        WU = [sb(ph, G, f"WU{i}", [128, KC, 512], BF16) for i in range(2)]
        WUr = [S.res(f"WU{i}") for i in range(2)]
        STG = [sb(ph, G, f"STG{i}", [128, 4, 512], F32) for i in range(4)]
        STGr = [S.res(f"STG{i}", dma=True) for i in range(4)]
        kctr = [0]
        asb = [sb(ph, G, f"asb{i}", [128, 2 + 2048], F32) for i in range(2)]
        asbr = [S.res(f"asb{i}") for i in range(2)]
        cbuf = [sb(ph, G, f"cbuf{i}", [128, 512], F32) for i in range(2)]
        cbufr = [S.res(f"cbuf{i}") for i in range(2)]
        gsb = [sb(ph, G, f"gsb{i}", [128, 512], BF16) for i in range(3)]
        gsbr = [S.res(f"gsb{i}", dma=True) for i in range(3)]
        wg, wu = G.inp["w_gate"][l], G.inp["w_up"][l]

        def ldw(fb):
            return (weight_steps2(G, WG[fb % 2], WGr[fb % 2], STG, STGr, kctr, wg[:, fb * 512:(fb + 1) * 512], KC, G.dres["w"]) +
                    weight_steps2(G, WU[fb % 2], WUr[fb % 2], STG, STGr, kctr, wu[:, fb * 512:(fb + 1) * 512], KC, G.dres["w"]))
        pend_cast = []
        for d_, c_ in ldw(0):
            d_()
            c_()
        it = 0
        wsteps = []
        for fb in range(11):
            if fb + 1 < 11:
                wsteps = ldw(fb + 1)
            for f4 in range(4):
                ft = fb * 4 + f4
                a = asb[ft % 2]
                ar = asbr[ft % 2]
                if hf == 0:
                    S.op("dve", lambda e, a=a: e.memset(a[:, 0:2], 0.0), writes=[ar])
                else:
                    S.op("dve", lambda e, a=a, ft=ft: e.tensor_copy(out=a[:, 0:2], in_=G.halo[:, ft, :]), reads=[G.halor], writes=[ar])
                for tg in range(4):
                    ba, bu = (it % 2) * 2, (it % 2) * 2 + 1
                    cbi = it % 2
                    gs = it % 3
                    it += 1
                    while pend_cast:
                        pend_cast.pop(0)()
                    if wsteps:
                        d_, c_ = wsteps.pop(0)
                        d_()
                        pend_cast.append(c_)
                    for kc in range(16):
                        S.op("pe", lambda e, kc=kc, f4=f4, tg=tg, ba=ba, fb=fb: e.matmul(
                            out=ps[ba][:], lhsT=WG[fb % 2][:, kc, f4 * 128:(f4 + 1) * 128], rhs=HT[:, kc, tg * 512:(tg + 1) * 512],
                            start=(kc == 0), stop=(kc == 15)), reads=[HTr, WGr[fb % 2]], writes=[psr[ba]])
                    for kc in range(16):
                        S.op("pe", lambda e, kc=kc, f4=f4, tg=tg, bu=bu, fb=fb: e.matmul(
                            out=ps[bu][:], lhsT=WU[fb % 2][:, kc, f4 * 128:(f4 + 1) * 128], rhs=HT[:, kc, tg * 512:(tg + 1) * 512],
                            start=(kc == 0), stop=(kc == 15)), reads=[HTr, WUr[fb % 2]], writes=[psr[bu]])
                    t0 = 2 + tg * 512
                    S.op("act", lambda e, a=a, t0=t0, ba=ba: e.activation(out=a[:, t0:t0 + 512], in_=ps[ba][:], func=AF.Copy),
                         reads=[psr[ba]], writes=[ar])
                    c = cbuf[cbi]
                    cr = cbufr[cbi]
                    S.op("dve", lambda e, a=a, t0=t0, c=c, ft=ft: e.tensor_scalar(out=c[:], in0=a[:, t0:t0 + 512], scalar1=cw[:, 2, ft:ft + 1],
                                                                              scalar2=cw[:, 3, ft:ft + 1], op0=ALU.mult, op1=ALU.add),
                         reads=[ar, cwr], writes=[cr])
                    S.op("dve", lambda e, a=a, t0=t0, c=c, ft=ft: e.scalar_tensor_tensor(out=c[:], in0=a[:, t0 - 1:t0 + 511], scalar=cw[:, 1, ft:ft + 1],
                                                                                     in1=c[:], op0=ALU.mult, op1=ALU.add),
                         reads=[ar, cwr, cr], writes=[cr])
                    S.op("dve", lambda e, a=a, t0=t0, c=c, ft=ft: e.scalar_tensor_tensor(out=c[:], in0=a[:, t0 - 2:t0 + 510], scalar=cw[:, 0, ft:ft + 1],
                                                                                     in1=c[:], op0=ALU.mult, op1=ALU.add),
                         reads=[ar, cwr, cr], writes=[cr])
                    S.op("act", lambda e, c=c: e.activation(out=c[:], in_=c[:], func=AF.Silu), reads=[cr], writes=[cr])
                    S.op("dve", lambda e, c=c, gs=gs, bu=bu: e.tensor_tensor(out=gsb[gs][:], in0=ps[bu][:], in1=c[:], op=ALU.mult),
                         reads=[psr[bu], cr], writes=[gsbr[gs]])
                    tok0 = hf * 2048 + tg * 512
                    S.dma("sp", G.scr["GT"][ft, :, tok0:tok0 + 512], gsb[gs][:], gsbr[gs], reads=[gsbr[gs]], writes=[G.dres["GT"]])
                if hf == 0:
                    S.op("dve", lambda e, a=a, ft=ft: e.tensor_copy(out=G.halo[:, ft, :], in_=a[:, 2048:2050]), reads=[ar], writes=[G.halor])
    run_phase(G, body)


def phase_H(G, l, hf, xdst):
    def body(ph):
        S = G.S
        ps = G.ps
        oap, ores = xdst
        WD = [sb(ph, G, f"WD{i}", [128, FT, 512], BF16) for i in range(2)]
        WDr = [S.res(f"WD{i}") for i in range(2)]
        STG = [sb(ph, G, f"STG{i}", [128, 4, 512], F32) for i in range(2)]
        STGr = [S.res(f"STG{i}", dma=True) for i in range(2)]
        kctr = [0]
        GB = [sb(ph, G, f"GB{i}", [128, FT, 512], BF16) for i in range(2)]
        GBr = [S.res(f"GB{i}", dma=True) for i in range(2)]
        xa = [sb(ph, G, f"xa{i}", [128, 512], F32) for i in range(4)]
        xar = [S.res(f"xa{i}", dma=True) for i in range(4)]
        psr = [S.res(f"ps{i}") for i in range(8)]
        wd = G.inp["w_down"][l]
        load_weight_block(G, WD[0], WDr[0], STG, STGr, kctr, wd[:, 0:512], FT, G.dres["w"])
        gi = 0

        def ldg(gidx):
            tg = gidx % 8
            tok0 = tg * 512
            S.dma("sp", GB[gidx % 2][:], G.scr["GT"][:, :, tok0:tok0 + 512].rearrange("f p t -> p f t"), GBr[gidx % 2],
                  reads=[G.dres["GT"]], writes=[GBr[gidx % 2]])
        ldg(0)
        iters = [(cb, tg, t4) for cb in range(4) for tg in range(8) for t4 in range(4)]

        def ldx(i):
            cb, tg, t4 = iters[i]
            gt = tg * 4 + t4
            S.dma("sp", xa[i % 4][:], G.scr["XR"][gt * 128:(gt + 1) * 128, cb * 512:(cb + 1) * 512], xar[i % 4],
                  reads=[G.dres["XR"]], writes=[xar[i % 4]])
        ldx(0)
        ldx(1)
        for it, (cb, tg, t4) in enumerate(iters):
            if tg == 0 and t4 == 0:
                wsteps = weight_steps(G, WD[(cb + 1) % 2], WDr[(cb + 1) % 2], STG, STGr, kctr, wd[:, (cb + 1) * 512:(cb + 2) * 512], FT, G.dres["w"]) if cb + 1 < 4 else []
            if wsteps:
                wsteps.pop(0)()
            if t4 == 0:
                if gi + 1 < 32:
                    ldg(gi + 1)
                gb, gbr = GB[gi % 2], GBr[gi % 2]
                gi += 1
            if it + 2 < len(iters):
                ldx(it + 2)
            w, wr = WD[cb % 2], WDr[cb % 2]
            gt = tg * 4 + t4
            bank = it % 4
            xs = it % 4
            for fc in range(FT):
                S.op("pe", lambda e, fc=fc, t4=t4, bank=bank, w=w, gb=gb: e.matmul(out=ps[bank][:], lhsT=gb[:, fc, t4 * 128:(t4 + 1) * 128],
                                                                                rhs=w[:, fc, :], start=(fc == 0), stop=(fc == FT - 1)),
                     reads=[gbr, wr], writes=[psr[bank]])
            S.op("dve", lambda e, xs=xs, bank=bank: e.tensor_tensor(out=xa[xs][:], in0=ps[bank][:], in1=xa[xs][:], op=ALU.add),
                 reads=[psr[bank], xar[xs]], writes=[xar[xs]])
            S.dma("sp", oap[gt * 128:(gt + 1) * 128, cb * 512:(cb + 1) * 512], xa[xs][:], xar[xs], reads=[xar[xs]], writes=[ores])
    run_phase(G, body)


def phase_C(G, l):
    def body(ph):
        S = G.S
        ps = G.ps
        psr = [S.res(f"ps{i}") for i in range(8)]
        cs, csr = {}, {}
        for k in ("ident", "dec", "qd"):
            cs[k] = sb(ph, G, "c_" + k, CONST_SHAPES[k][0], CONST_SHAPES[k][1])
            csr[k] = S.res("c_" + k, dma=True)
            S.dma("sp", cs[k][:], G.cst[k], csr[k], writes=[csr[k]])
        gr = sb(ph, G, "gr", [128, 1024], F32)
        grr = S.res("gr", dma=True)
        S.dma("sp", gr[:], G.inp["ret_norm_g"][l].partition_broadcast(128), grr, writes=[grr])
        qT = [sb(ph, G, f"qT{i}", [128, 2, 1024], BF16) for i in range(2)]
        kT = [sb(ph, G, f"kT{i}", [128, 2, 1024], BF16) for i in range(2)]
        kk = [sb(ph, G, f"kk{i}", [128, 8, 256], BF16) for i in range(2)]
        vv = [sb(ph, G, f"vv{i}", [128, 8, 256], BF16) for i in range(2)]
        sg = [sb(ph, G, f"sg{i}", [128, 8, 256], F32) for i in range(2)]
        qTr = [S.res(f"qT{i}", dma=True) for i in range(2)]
        kTr = [S.res(f"kT{i}", dma=True) for i in range(2)]
        kkr = [S.res(f"kk{i}", dma=True) for i in range(2)]
        vvr = [S.res(f"vv{i}", dma=True) for i in range(2)]
        sgr = [S.res(f"sg{i}", dma=True) for i in range(2)]
        st32 = sb(ph, G, "st32", [128, 512], F32)
        st32r = S.res("st32")
        stb = [sb(ph, G, f"stb{i}", [128, 2, 256], BF16) for i in range(2)]
        stbr = [S.res(f"stb{i}") for i in range(2)]
        PT = [sb(ph, G, f"PT{i}", [128, 2, 256], BF16) for i in range(2)]
        PTr = [S.res(f"PT{i}") for i in range(2)]
        ytmp = [sb(ph, G, f"ytmp{i}", [128, 512], F32) for i in range(2)]
        ytmpr = [S.res(f"ytmp{i}") for i in range(2)]
        yy = [sb(ph, G, f"yy{i}", [128, 512], F32) for i in range(2)]
        yyr = [S.res(f"yy{i}") for i in range(2)]
        t2 = [sb(ph, G, f"t2{i}", [128, 512], F32) for i in range(2)]
        t2r = [S.res(f"t2{i}") for i in range(2)]
        zz = [sb(ph, G, f"zz{i}", [128, 512], BF16) for i in range(2)]
        zzr = [S.res(f"zz{i}") for i in range(2)]
        junk = sb(ph, G, "junkc", [128, 256], BF16)
        junkr = S.res("junkc")
        sm = sb(ph, G, "smc", [128, 16], F32)
        smr = [S.res(f"smc{i}") for i in range(2)]
        yts = [sb(ph, G, f"yts{i}", [128, 2, 1024], BF16) for i in range(2)]
        ytsr = [S.res(f"yts{i}", dma=True) for i in range(2)]

        def load_group(h, g, s):
            t0 = g * 1024
            S.dma("sp", qT[s][:], G.scr["RQT"][h * 2:h * 2 + 2, :, t0:t0 + 1024].rearrange("c p t -> p c t"), qTr[s], reads=[G.dres["RQT"]], writes=[qTr[s]])
            S.dma("sp", kT[s][:], G.scr["RKT"][h * 2:h * 2 + 2, :, t0:t0 + 1024].rearrange("c p t -> p c t"), kTr[s], reads=[G.dres["RKT"]], writes=[kTr[s]])
            S.dma("sp", kk[s][:], G.scr["RK"][t0:t0 + 1024, h * 256:(h + 1) * 256].rearrange("(t p) c -> p t c", p=128), kkr[s], reads=[G.dres["RK"]], writes=[kkr[s]])
            S.dma("sp", vv[s][:], G.scr["RV"][t0:t0 + 1024, h * 256:(h + 1) * 256].rearrange("(t p) c -> p t c", p=128), vvr[s], reads=[G.dres["RV"]], writes=[vvr[s]])
            S.dma("sp", sg[s][:], G.scr["SG"][t0:t0 + 1024, h * 256:(h + 1) * 256].rearrange("(t p) c -> p t c", p=128), sgr[s], reads=[G.dres["SG"]], writes=[sgr[s]])
        groups = [(h, g) for h in range(4) for g in range(4)]
        chunks = [(h, n) for h in range(4) for n in range(16)]
        stb3 = stb + [sb(ph, G, "stb2", [128, 2, 256], BF16)]
        stb3r = stbr + [S.res("stb2")]

        def stage1(c):
            h, n = chunks[c]
            s, n4, i2 = (c // 4) % 2, n % 4, c % 2
            lt0 = n4 * 2
            for si in range(2):
                for dc in range(2):
                    S.op("pe", lambda e, si=si, dc=dc: e.matmul(
                        out=ps[i2][:, si * 256:(si + 1) * 256], lhsT=kT[s][:, dc, (lt0 + si) * 128:(lt0 + si + 1) * 128],
                        rhs=qT[s][:, dc, n4 * 256:(n4 + 1) * 256], start=(dc == 0), stop=(dc == 1)),
                        reads=[kTr[s], qTr[s]], writes=[psr[i2]])
            S.op("dve", lambda e: e.tensor_tensor(out=PT[i2][:].rearrange("p a c -> p (a c)"), in0=ps[i2][:],
                                                  in1=cs["dec"][:, h].rearrange("p a c -> p (a c)"), op=ALU.mult),
                 reads=[psr[i2], csr["dec"]], writes=[PTr[i2]])
            if n < 15:
                cdec = float(np.float32(GAMMA[h]) ** 256)
                for dc in range(2):
                    for si in range(2):
                        S.op("pe", lambda e, dc=dc, si=si: e.matmul(
                            out=ps[4][:, dc * 256:(dc + 1) * 256], lhsT=kk[s][:, lt0 + si, dc * 128:(dc + 1) * 128], rhs=vv[s][:, lt0 + si, :],
                            start=(si == 0), stop=(si == 1)), reads=[kkr[s], vvr[s]], writes=[psr[4]])
                if n == 0:
                    S.op("dve", lambda e: e.tensor_copy(out=st32[:], in_=ps[4][:]), reads=[psr[4]], writes=[st32r])
                else:
                    S.op("dve", lambda e: e.scalar_tensor_tensor(out=st32[:], in0=st32[:], scalar=cdec, in1=ps[4][:], op0=ALU.mult, op1=ALU.add),
                         reads=[psr[4], st32r], writes=[st32r])
                nx = (c + 1) % 3
                S.op("pool", lambda e: e.tensor_copy(out=stb3[nx][:].rearrange("p a c -> p (a c)"), in_=st32[:]), reads=[st32r], writes=[stb3r[nx]])

        def stage2(c):
            h, n = chunks[c]
            s, n4, i2 = (c // 4) % 2, n % 4, c % 2
            lt0 = n4 * 2
            cur = c % 3
            for ci in range(2):
                for si in range(ci + 1):
                    S.op("pe", lambda e, ci=ci, si=si: e.matmul(
                        out=ps[2][:, ci * 256:(ci + 1) * 256], lhsT=PT[i2][:, si, ci * 128:(ci + 1) * 128], rhs=vv[s][:, lt0 + si, :],
                        start=(si == 0), stop=(si == ci)), reads=[PTr[i2], vvr[s]], writes=[psr[2]])
            if n > 0:
                for ci in range(2):
                    for dc in range(2):
                        S.op("pe", lambda e, ci=ci, dc=dc: e.matmul(
                            out=ps[3][:, ci * 256:(ci + 1) * 256], lhsT=qT[s][:, dc, n4 * 256 + ci * 128:n4 * 256 + (ci + 1) * 128],
                            rhs=stb3[cur][:, dc, :], start=(dc == 0), stop=(dc == 1)), reads=[qTr[s], stb3r[cur]], writes=[psr[3]])
                for ci in range(2):
                    S.op("act", lambda e, ci=ci: e.activation(out=ytmp[i2][:, ci * 256:(ci + 1) * 256], in_=ps[3][:, ci * 256:(ci + 1) * 256],
                                                              func=AF.Copy, scale=cs["qd"][:, ci, h:h + 1]),
                         reads=[psr[3], csr["qd"]], writes=[ytmpr[i2]])
                S.op("dve", lambda e: e.tensor_tensor(out=yy[i2][:], in0=ps[2][:], in1=ytmp[i2][:], op=ALU.add),
                     reads=[psr[2], ytmpr[i2]], writes=[yyr[i2]])
            else:
                S.op("dve", lambda e: e.tensor_copy(out=yy[i2][:], in_=ps[2][:]), reads=[psr[2]], writes=[yyr[i2]])
            c0 = i2 * 4
            for ci in range(2):
                S.op("act", lambda e, ci=ci: e.activation(out=junk[:], in_=yy[i2][:, ci * 256:(ci + 1) * 256], func=AF.Square,
                                                          accum_out=sm[:, c0 + ci:c0 + ci + 1]),
                     reads=[yyr[i2]], writes=[junkr, smr[i2]])
            S.op("act", lambda e: e.activation(out=sm[:, c0 + 2:c0 + 4], in_=sm[:, c0:c0 + 2], func=AF.Ln, scale=1.0 / 256, bias=EPS),
                 reads=[smr[i2]], writes=[smr[i2]])
            S.op("act", lambda e: e.activation(out=sm[:, c0 + 2:c0 + 4], in_=sm[:, c0 + 2:c0 + 4], func=AF.Exp, scale=-0.5),
                 reads=[smr[i2]], writes=[smr[i2]])
            for ci in range(2):
                S.op("pool", lambda e, ci=ci: e.tensor_tensor(out=t2[i2][:, ci * 256:(ci + 1) * 256], in0=sg[s][:, lt0 + ci, :],
                                                              in1=gr[:, h * 256:(h + 1) * 256], op=ALU.mult),
                     reads=[sgr[s], grr], writes=[t2r[i2]])
            for ci in range(2):
                S.op("dve", lambda e, ci=ci: e.scalar_tensor_tensor(out=zz[i2][:, ci * 256:(ci + 1) * 256], in0=yy[i2][:, ci * 256:(ci + 1) * 256],
                                                                    scalar=sm[:, c0 + 2 + ci:c0 + 3 + ci], in1=t2[i2][:, ci * 256:(ci + 1) * 256],
                                                                    op0=ALU.mult, op1=ALU.mult),
                     reads=[yyr[i2], smr[i2], t2r[i2]], writes=[zzr[i2]])

        def stage3(c):
            h, n = chunks[c]
            s, n4, i2 = (c // 4) % 2, n % 4, c % 2
            trb = 5 + i2
            pv = ps[trb][:].bitcast(BF16)
            for ec in range(2):
                for ci in range(2):
                    S.op("pe", lambda e, ec=ec, ci=ci: e.transpose(out=pv[:, ec * 256 + ci * 128:ec * 256 + (ci + 1) * 128],
                                                                   in_=zz[i2][:, ci * 256 + ec * 128:ci * 256 + (ec + 1) * 128], identity=cs["ident"][:]),
                         reads=[zzr[i2], csr["ident"]], writes=[psr[trb]])
            S.op("act", lambda e: e.activation(out=yts[s][:, :, n4 * 256:(n4 + 1) * 256], in_=pv[:, 0:512].rearrange("p (a t) -> p a t", a=2),
                                               func=AF.Copy), reads=[psr[trb]], writes=[ytsr[s]])
            if n4 == 3:
                g = n // 4
                S.dma("sp", G.scr["YTD"][h * 2:h * 2 + 2, :, g * 1024:(g + 1) * 1024].rearrange("c p t -> p c t"), yts[s][:], ytsr[s],
                      reads=[ytsr[s]], writes=[G.dres["YTD"]])
        load_group(groups[0][0], groups[0][1], 0)
        load_group(groups[1][0], groups[1][1], 1)
        NCH = len(chunks)
        stage1(0)
        for c in range(NCH):
            if c + 1 < NCH:
                stage1(c + 1)
            stage2(c)
            if c % 4 == 3 and c // 4 + 2 < len(groups):
                gg = c // 4 + 2
                load_group(groups[gg][0], groups[gg][1], gg % 2)
            if c >= 1:
                stage3(c - 1)
        stage3(NCH - 1)
    run_phase(G, body)


def phase_D(G, l):
    def body(ph):
        S = G.S
        ps = G.ps
        psr = [S.res(f"ps{i}") for i in range(8)]
        cs, csr = {}, {}
        for k in ("ident", "ones", "bl", "tri", "negb", "validf", "own1h"):
            cs[k] = sb(ph, G, "c_" + k, CONST_SHAPES[k][0], CONST_SHAPES[k][1])
            csr[k] = S.res("c_" + k, dma=True)
            S.dma("sp", cs[k][:], G.cst[k], csr[k], writes=[csr[k]])
        qT = [sb(ph, G, f"mq{i}", [128, T], BF16) for i in range(2)]
        kT = [sb(ph, G, f"mk{i}", [128, T], BF16) for i in range(2)]
        vv = [sb(ph, G, f"mv{i}", [128, 32, 128], BF16) for i in range(2)]
        brhs = [sb(ph, G, f"brhs{i}", [128, T], BF16) for i in range(2)]
        qTr = [S.res(f"mq{i}", dma=True) for i in range(2)]
        kTr = [S.res(f"mk{i}", dma=True) for i in range(2)]
        vvr = [S.res(f"mv{i}", dma=True) for i in range(2)]
        brr = [S.res(f"brhs{i}", dma=True) for i in range(2)]
        km32 = sb(ph, G, "km32", [128, 16], F32)
        kmb = sb(ph, G, "kmb", [128, 2, 16], BF16)
        kmr = S.res("km")
        gsel = sb(ph, G, "gsel", [128, 512], F32)
        sel = sb(ph, G, "sel", [128, 512], F32)
        top8 = sb(ph, G, "top8", [128, 256], F32)
        maskb = sb(ph, G, "maskb", [128, 512], BF16)
        selr = S.res("sel")
        PT = [sb(ph, G, f"PTm{i}", [128, 512], BF16) for i in range(3)]
        PTr = [S.res(f"PTm{i}") for i in range(3)]
        rden = sb(ph, G, "rden", [128, 512], F32)
        rdenr = S.res("rden")
        yo = [sb(ph, G, f"yo{i}", [128, 512], BF16) for i in range(2)]
        yor = [S.res(f"yo{i}", dma=True) for i in range(2)]
        for i in range(2):
            S.op("pool", lambda e, i=i: e.memset(brhs[i][:], 0.0), writes=[brr[i]])

        def load_head(h, s):
            S.dma("sp", qT[s][:], G.scr["MQT"][h], qTr[s], reads=[G.dres["MQT"]], writes=[qTr[s]])
            S.dma("sp", kT[s][:], G.scr["MKT"][h], kTr[s], reads=[G.dres["MKT"]], writes=[kTr[s]])
            S.dma("sp", vv[s][:], G.scr["MV"][:, h * 128:(h + 1) * 128].rearrange("(t p) d -> p t d", p=128), vvr[s], reads=[G.dres["MV"]], writes=[vvr[s]])
            S.dma("sp", brhs[s][32:36, :], G.cst["brc"][h], brr[s], writes=[brr[s]])
        pv7 = ps[7][:].bitcast(BF16)

        def ms_a(h):
            s = h % 2
            S.op("dve", lambda e: e.tensor_reduce(out=km32[:], in_=kT[s][:].rearrange("p (b t) -> p b t", b=16), axis=AX.X, op=ALU.add),
                 reads=[kTr[s]], writes=[kmr])
            S.op("dve", lambda e: e.tensor_scalar(out=km32[:], in0=km32[:], scalar1=1.0 / 256, scalar2=None, op0=ALU.mult), reads=[kmr], writes=[kmr])
            S.op("dve", lambda e: e.tensor_copy(out=kmb[:, 0, :], in_=km32[:]), reads=[kmr], writes=[kmr])
            S.op("dve", lambda e: e.tensor_tensor(out=kmb[:, 1, :], in0=km32[:], in1=kmb[:, 0, :], op=ALU.subtract), reads=[kmr], writes=[kmr])

        def ms_b(h):
            s = h % 2
            for qt in range(32):
                for a_ in range(2):
                    S.op("pe", lambda e, qt=qt, a_=a_: e.matmul(out=ps[6][:, qt * 16:(qt + 1) * 16], lhsT=qT[s][:, qt * 128:(qt + 1) * 128],
                                                               rhs=kmb[:, a_, :], start=(a_ == 0), stop=(a_ == 1)),
                         reads=[qTr[s], kmr], writes=[psr[6]])
            S.op("dve", lambda e: e.tensor_tensor(out=gsel[:], in0=ps[6][:], in1=cs["negb"][:].rearrange("p a b -> p (a b)"), op=ALU.add),
                 reads=[psr[6], csr["negb"]], writes=[selr])
            for qt in range(32):
                S.op("dve", lambda e, qt=qt: e.max(out=top8[:, qt * 8:(qt + 1) * 8], in_=gsel[:, qt * 16:(qt + 1) * 16]), reads=[selr], writes=[selr])
            for qt in range(32):
                S.op("dve", lambda e, qt=qt: e.tensor_scalar(out=sel[:, qt * 16:(qt + 1) * 16], in0=gsel[:, qt * 16:(qt + 1) * 16],
                                                             scalar1=top8[:, qt * 8 + 2:qt * 8 + 3], scalar2=None, op0=ALU.is_ge),
                     reads=[selr], writes=[selr])
            S.op("dve", lambda e: e.tensor_tensor(out=sel[:], in0=sel[:], in1=cs["validf"][:].rearrange("p a b -> p (a b)"), op=ALU.mult),
                 reads=[selr, csr["validf"]], writes=[selr])
            S.op("dve", lambda e: e.tensor_tensor(out=sel[:], in0=sel[:], in1=cs["own1h"][:].rearrange("p a b -> p (a b)"), op=ALU.add),
                 reads=[selr, csr["own1h"]], writes=[selr])
            S.op("dve", lambda e: e.tensor_scalar(out=maskb[:], in0=sel[:], scalar1=-1.0, scalar2=-NEG, op0=ALU.add, op1=ALU.mult),
                 reads=[selr], writes=[selr])

        def ms_c(h):
            s = h % 2
            for grp in range(4):
                for q8 in range(8):
                    qt = grp * 8 + q8
                    S.op("pe", lambda e, qt=qt, q8=q8: e.transpose(out=pv7[0:16, q8 * 128:(q8 + 1) * 128], in_=maskb[:, qt * 16:(qt + 1) * 16],
                                                                   identity=cs["ident"][:]),
                         reads=[selr, csr["ident"]], writes=[psr[7]])
                S.op("act", lambda e, grp=grp: e.activation(out=brhs[s][0:16, grp * 1024:(grp + 1) * 1024], in_=pv7[0:16, 0:1024], func=AF.Copy),
                     reads=[psr[7]], writes=[brr[s]])
        load_head(0, 0)
        ms_a(0)
        ms_b(0)
        ms_c(0)
        pi = 0
        for h in range(8):
            s = h % 2
            if h + 1 < 8:
                load_head(h + 1, (h + 1) % 2)
            for Q in range(8):
                nkt = 4 * Q + 4
                ob, db = 2 + Q % 2, 4 + Q % 2
                pend = None

                def pv_mm(j, pt, ob=ob, db=db, nkt=nkt, s=s):
                    S.op("pe", lambda e: e.matmul(out=ps[ob][:], lhsT=vv[s][:, j, :], rhs=PT[pt][:], start=(j == 0), stop=(j == nkt - 1)),
                         reads=[vvr[s], PTr[pt]], writes=[psr[ob]])
                    S.op("pe", lambda e: e.matmul(out=ps[db][:], lhsT=cs["ones"][:], rhs=PT[pt][:], start=(j == 0), stop=(j == nkt - 1)),
                         reads=[csr["ones"], PTr[pt]], writes=[psr[db]])
                for j in range(nkt):
                    scb = pi % 2
                    pt = pi % 3
                    pi += 1
                    diag = j >= 4 * Q
                    S.op("pe", lambda e, j=j, Q=Q, scb=scb, s=s: e.matmul(out=ps[scb][:], lhsT=kT[s][:, j * 128:(j + 1) * 128], rhs=qT[s][:, Q * 512:(Q + 1) * 512],
                                                                         start=True, stop=False), reads=[kTr[s], qTr[s]], writes=[psr[scb]])
                    S.op("pe", lambda e, j=j, Q=Q, scb=scb, s=s, diag=diag: e.matmul(out=ps[scb][:], lhsT=cs["bl"][:, j, :], rhs=brhs[s][:, Q * 512:(Q + 1) * 512],
                                                                                    start=False, stop=(not diag)), reads=[csr["bl"], brr[s]], writes=[psr[scb]])
                    if diag:
                        c0 = (j // 2 - 2 * Q) * 256
                        S.op("pe", lambda e, j=j, scb=scb, c0=c0: e.matmul(out=ps[scb][:, c0:c0 + 256], lhsT=cs["ident"][:], rhs=cs["tri"][:, j % 2, :],
                                                                          start=False, stop=True), reads=[csr["ident"], csr["tri"]], writes=[psr[scb]])
                    S.op("act", lambda e, scb=scb, pt=pt: e.activation(out=PT[pt][:], in_=ps[scb][:], func=AF.Exp), reads=[psr[scb]], writes=[PTr[pt]])
                    if pend is not None:
                        pv_mm(*pend)
                    pend = (j, pt)
                pv_mm(*pend)
                S.op("dve", lambda e, db=db: e.reciprocal(out=rden[:], in_=ps[db][:]), reads=[psr[db]], writes=[rdenr])
                S.op("dve", lambda e, ob=ob, Q=Q: e.tensor_tensor(out=yo[Q % 2][:], in0=ps[ob][:], in1=rden[:], op=ALU.mult),
                     reads=[psr[ob], rdenr], writes=[yor[Q % 2]])
                S.dma("sp", G.scr["YTD"][8 + h, :, Q * 512:(Q + 1) * 512], yo[Q % 2][:], yor[Q % 2], reads=[yor[Q % 2]], writes=[G.dres["YTD"]])
                if h + 1 < 8:
                    if Q == 1:
                        ms_a(h + 1)
                    elif Q == 3:
                        ms_b(h + 1)
                    elif Q == 5:
                        ms_c(h + 1)
    run_phase(G, body)
```
